# Optimizing a Trainium2 kernel written in Bass

```python
import jax
import jax.numpy as jnp
from jax import lax
import numpy as np


D_MODEL = 1024
BATCH = 4
SEQ = 4096
DEPTH = 2

GRID_W = 64
CTX_LEN = 256
MLA_HEADS = 8
MLA_NOPE = 64
MLA_ROPE = 32
MLA_QK = MLA_NOPE + MLA_ROPE
MLA_V = 64
Q_LORA = 256
KV_LORA = 128
GQA_HEADS = 8
GQA_KV_HEADS = 2
GQA_GROUP = GQA_HEADS // GQA_KV_HEADS
GQA_HD = 64
N_EXPERTS = 16
N_GROUPS = 4
EXPERTS_PER_GROUP = N_EXPERTS // N_GROUPS
TOP_K = 2
FF_EXPERT = 512
FF_SHARED = 512
ROPE_THETA = 10000.0
EPS = 1e-6
Q_BLOCK = 128
IN_SIZES = (Q_LORA, KV_LORA, MLA_ROPE, GQA_HEADS * GQA_HD, GQA_KV_HEADS * GQA_HD, GQA_KV_HEADS * GQA_HD, 2 * D_MODEL)
IN_COLS = Q_LORA + KV_LORA + MLA_ROPE + GQA_HEADS * GQA_HD + 2 * GQA_KV_HEADS * GQA_HD + 2 * D_MODEL

kernel_name = 'hybrid_mla_gqa_grouped_moe_dit_block'


def rmsnorm(x, g):
    xf = x.astype(jnp.float32)
    y = xf * lax.rsqrt(jnp.mean(xf * xf, axis=-1, keepdims=True) + EPS)
    return (y * g.astype(jnp.float32)).astype(x.dtype)


def modulate(h, shift, scale):
    return h * (1 + scale[:, None, :]) + shift[:, None, :]


def axial_angles(pos, half_dim):
    inv = ROPE_THETA ** (-jnp.arange(0, half_dim, 2, dtype=jnp.float32) / half_dim)
    return pos.astype(jnp.float32)[:, None] * inv[None, :]


def rotate(x, ang):
    cos = jnp.cos(ang)[None, :, None, :].astype(x.dtype)
    sin = jnp.sin(ang)[None, :, None, :].astype(x.dtype)
    x1, x2 = jnp.split(x, 2, axis=-1)
    return jnp.concatenate([x1 * cos - x2 * sin, x1 * sin + x2 * cos], axis=-1)


def rope_2d(x, ang_row, ang_col):
    xr, xc = jnp.split(x, 2, axis=-1)
    return jnp.concatenate([rotate(xr, ang_row), rotate(xc, ang_col)], axis=-1)


def attend(q, k, v, scale):
    b, n, hk, g, d = q.shape
    nb = n // Q_BLOCK
    qb = q.reshape(b, nb, Q_BLOCK, hk, g, d).transpose(1, 0, 2, 3, 4, 5)

    def one_block(qi):
        s = jnp.einsum('bqhgd,bkhd->bhgqk', qi, k).astype(jnp.float32) * scale
        p = jax.nn.softmax(s, axis=-1).astype(v.dtype)
        return jnp.einsum('bhgqk,bkhe->bqhge', p, v)

    o = lax.map(one_block, qb)
    return o.transpose(1, 0, 2, 3, 4, 5).reshape(b, n, hk * g * v.shape[-1])


def mixer_inputs(h, w_in_l, g_cq_l, w_uq_l, g_ckv_l, w_ukv_l, g_qa_l, g_ka_l, g_qb_l, g_kb_l, angles):
    b, n, _ = h.shape
    proj = h @ w_in_l
    splits = [int(s) for s in np.cumsum(IN_SIZES)[:-1]]
    cq, ckv, kr, qb, kb, vb, gts = jnp.split(proj, splits, axis=-1)
    qa = (rmsnorm(cq, g_cq_l) @ w_uq_l).reshape(b, n, MLA_HEADS, MLA_QK)
    kva = (rmsnorm(ckv, g_ckv_l) @ w_ukv_l).reshape(b, n, MLA_HEADS, MLA_NOPE + MLA_V)
    k_nope, va = jnp.split(kva, [MLA_NOPE], axis=-1)
    k_rope = jnp.broadcast_to(kr[:, :, None, :], (b, n, MLA_HEADS, MLA_ROPE))
    ka = jnp.concatenate([k_nope, k_rope], axis=-1)
    qa = rmsnorm(qa, g_qa_l)
    ka = rmsnorm(ka, g_ka_l)
    qb = rmsnorm(qb.reshape(b, n, GQA_HEADS, GQA_HD), g_qb_l)
    kb = rmsnorm(kb.reshape(b, n, GQA_KV_HEADS, GQA_HD), g_kb_l)
    vb = vb.reshape(b, n, GQA_KV_HEADS, GQA_HD)
    if angles is not None:
        ar_a, ac_a, ar_b, ac_b = angles
        qa = jnp.concatenate([qa[..., :MLA_NOPE], rope_2d(qa[..., MLA_NOPE:], ar_a, ac_a)], axis=-1)
        ka = jnp.concatenate([ka[..., :MLA_NOPE], rope_2d(ka[..., MLA_NOPE:], ar_a, ac_a)], axis=-1)
        qb = rope_2d(qb, ar_b, ac_b)
        kb = rope_2d(kb, ar_b, ac_b)
    qa = qa[:, :, :, None, :]
    qb = qb.reshape(b, n, GQA_KV_HEADS, GQA_GROUP, GQA_HD)
    ga, gb = jnp.split(gts, 2, axis=-1)
    return (qa, ka, va, qb, kb, vb, ga, gb)


def merge_branches(oa, ob, ga, gb, w_oa_l, w_ob_l, w_out_l):
    y = jax.nn.sigmoid(ga) * (oa @ w_oa_l) + jax.nn.sigmoid(gb) * (ob @ w_ob_l)
    return y @ w_out_l


def swiglu(h, wg, wu, wd):
    return (jax.nn.silu(h @ wg) * (h @ wu)) @ wd


def moe(h, w_router, b_router, wg, wu, wd, sg, su, sd):
    scores = jax.nn.sigmoid(jnp.einsum('bnd,de->bne', h, w_router).astype(jnp.float32))
    sel = scores + b_router.astype(jnp.float32)
    grp = sel.reshape(sel.shape[:-1] + (N_GROUPS, EXPERTS_PER_GROUP))
    grp_score = lax.top_k(grp, TOP_K)[0].sum(axis=-1)
    best = jnp.argmax(grp_score, axis=-1)
    gmask = jnp.arange(N_GROUPS)[None, None, :] == best[..., None]
    masked = jnp.where(gmask[..., None], grp, -jnp.inf).reshape(sel.shape)
    _, idx = lax.top_k(masked, TOP_K)
    w = jnp.take_along_axis(scores, idx, axis=-1)
    w = w / jnp.sum(w, axis=-1, keepdims=True)
    gate = jnp.sum(jax.nn.one_hot(idx, N_EXPERTS, dtype=jnp.float32) * w[..., None], axis=-2).astype(h.dtype)
    y = swiglu(h, sg, su, sd)
    for e in range(N_EXPERTS):
        y = y + gate[..., e:e + 1] * swiglu(h, wg[e], wu[e], wd[e])
    return y


def setup_inputs(seed: int = 0) -> dict:
    key = jax.random.key(seed)
    ks = jax.random.split(key, 32)
    f32 = jnp.float32

    def nrm(k, shape, fan_in, mult=1.0):
        return jax.random.normal(k, shape, f32) * (mult * fan_in ** -0.5)

    def gain(k, shape):
        return 1.0 + 0.02 * jax.random.normal(k, shape, f32)

    L = DEPTH
    return {
        'x': jax.random.normal(ks[0], (BATCH, SEQ, D_MODEL), f32),
        'c': jax.random.normal(ks[1], (BATCH, D_MODEL), f32),
        'ctx': jax.random.normal(ks[2], (BATCH, CTX_LEN, D_MODEL), f32),
        'c_ctx': jax.random.normal(ks[3], (D_MODEL,), f32),
        'w_mod': nrm(ks[4], (L, D_MODEL, 6 * D_MODEL), D_MODEL, 0.5),
        'b_mod': 0.02 * jax.random.normal(ks[5], (L, 6 * D_MODEL), f32),
        'g_attn': gain(ks[6], (L, D_MODEL)),
        'g_ffn': gain(ks[7], (L, D_MODEL)),
        'w_in': nrm(ks[8], (L, D_MODEL, IN_COLS), D_MODEL),
        'g_cq': gain(ks[9], (L, Q_LORA)),
        'w_uq': nrm(ks[10], (L, Q_LORA, MLA_HEADS * MLA_QK), Q_LORA),
        'g_ckv': gain(ks[11], (L, KV_LORA)),
        'w_ukv': nrm(ks[12], (L, KV_LORA, MLA_HEADS * (MLA_NOPE + MLA_V)), KV_LORA),
        'g_qa': gain(ks[13], (L, MLA_QK)),
        'g_ka': gain(ks[14], (L, MLA_QK)),
        'g_qb': gain(ks[15], (L, GQA_HD)),
        'g_kb': gain(ks[16], (L, GQA_HD)),
        'w_oa': nrm(ks[17], (L, MLA_HEADS * MLA_V, D_MODEL), MLA_HEADS * MLA_V),
        'w_ob': nrm(ks[18], (L, GQA_HEADS * GQA_HD, D_MODEL), GQA_HEADS * GQA_HD),
        'w_out': nrm(ks[19], (L, D_MODEL, D_MODEL), D_MODEL),
        'w_router': nrm(ks[20], (D_MODEL, N_EXPERTS), D_MODEL),
        'b_router': 0.01 * jax.random.normal(ks[21], (N_EXPERTS,), f32),
        'w_e_gate': nrm(ks[22], (L, N_EXPERTS, D_MODEL, FF_EXPERT), D_MODEL),
        'w_e_up': nrm(ks[23], (L, N_EXPERTS, D_MODEL, FF_EXPERT), D_MODEL),
        'w_e_down': nrm(ks[24], (L, N_EXPERTS, FF_EXPERT, D_MODEL), FF_EXPERT),
        'w_s_gate': nrm(ks[25], (L, D_MODEL, FF_SHARED), D_MODEL),
        'w_s_up': nrm(ks[26], (L, D_MODEL, FF_SHARED), D_MODEL),
        'w_s_down': nrm(ks[27], (L, FF_SHARED, D_MODEL), FF_SHARED),
    }


def reference(x, c, ctx, c_ctx, w_mod, b_mod, g_attn, g_ffn, w_in, g_cq, w_uq, g_ckv, w_ukv,
              g_qa, g_ka, g_qb, g_kb, w_oa, w_ob, w_out, w_router, b_router,
              w_e_gate, w_e_up, w_e_down, w_s_gate, w_s_up, w_s_down):
    n_lat = x.shape[1]
    n_ctx = ctx.shape[1]
    rows_n = n_lat // GRID_W
    rows = jnp.broadcast_to(jnp.arange(rows_n)[:, None], (rows_n, GRID_W)).reshape(-1)
    cols = jnp.broadcast_to(jnp.arange(GRID_W)[None, :], (rows_n, GRID_W)).reshape(-1)
    angles = (axial_angles(rows, MLA_ROPE // 2), axial_angles(cols, MLA_ROPE // 2),
              axial_angles(rows, GQA_HD // 2), axial_angles(cols, GQA_HD // 2))
    scale_a = MLA_QK ** -0.5
    scale_b = GQA_HD ** -0.5

    for l in range(DEPTH):
        last = l == DEPTH - 1
        mod_l = jax.nn.silu(c) @ w_mod[l] + b_mod[l]
        mod_c = jax.nn.silu(c_ctx)[None, :] @ w_mod[l] + b_mod[l]
        sh1_l, sc1_l, gt1_l, sh2_l, sc2_l, gt2_l = jnp.split(mod_l, 6, axis=-1)
        sh1_c, sc1_c, gt1_c, sh2_c, sc2_c, gt2_c = jnp.split(mod_c, 6, axis=-1)
        wl = (w_in[l], g_cq[l], w_uq[l], g_ckv[l], w_ukv[l], g_qa[l], g_ka[l], g_qb[l], g_kb[l])

        h_l = modulate(rmsnorm(x, g_attn[l]), sh1_l, sc1_l)
        h_c = modulate(rmsnorm(ctx, g_attn[l]), sh1_c, sc1_c)
        qa_l, ka_l, va_l, qb_l, kb_l, vb_l, ga_l, gb_l = mixer_inputs(h_l, *wl, angles)
        qa_c, ka_c, va_c, qb_c, kb_c, vb_c, ga_c, gb_c = mixer_inputs(h_c, *wl, None)
        oa_l = attend(qa_l, jnp.concatenate([ka_c, ka_l], axis=1), jnp.concatenate([va_c, va_l], axis=1), scale_a)
        ob_l = attend(qb_l, jnp.concatenate([kb_c, kb_l], axis=1), jnp.concatenate([vb_c, vb_l], axis=1), scale_b)
        x_new = x + gt1_l[:, None, :] * merge_branches(oa_l, ob_l, ga_l, gb_l, w_oa[l], w_ob[l], w_out[l])
        if not last:
            oa_c = attend(qa_c, ka_c, va_c, scale_a)
            ob_c = attend(qb_c, kb_c, vb_c, scale_b)
            ctx = ctx + gt1_c[:, None, :] * merge_branches(oa_c, ob_c, ga_c, gb_c, w_oa[l], w_ob[l], w_out[l])
        x = x_new

        e_args = (w_router, b_router, w_e_gate[l], w_e_up[l], w_e_down[l], w_s_gate[l], w_s_up[l], w_s_down[l])
        f_l = modulate(rmsnorm(x, g_ffn[l]), sh2_l, sc2_l)
        if not last:
            f_c = modulate(rmsnorm(ctx, g_ffn[l]), sh2_c, sc2_c)
            y_all = moe(jnp.concatenate([f_c, f_l], axis=1), *e_args)
            ctx = ctx + gt2_c[:, None, :] * y_all[:, :n_ctx]
            x = x + gt2_l[:, None, :] * y_all[:, n_ctx:]
        else:
            x = x + gt2_l[:, None, :] * moe(f_l, *e_args)
    return x
```

```python
import numpy as np
import ml_dtypes
from contextlib import ExitStack
import concourse.bass as bass
import concourse.mybir as mybir
from concourse.bass_utils import run_bass_kernel_spmd

F32 = mybir.dt.float32
BF16 = mybir.dt.bfloat16
I32 = mybir.dt.int32
AF = mybir.ActivationFunctionType
ALU = mybir.AluOpType
AX = mybir.AxisListType

D = 1024
NB = 4
SEQ = 4096
CTX = 256
NCORES = 8
NLAT_T = 16
NT = 17
NTOK = NT * 128
NKEY = SEQ + CTX
NKT = NKEY // 128
EPS = 1e-6
THETA = 10000.0
IN_COLS = 3232
C_CQ, C_CKV, C_KR, C_QB, C_KB, C_VB, C_GA = 0, 256, 384, 416, 928, 1056, 1184
SCALE_A = 96 ** -0.5
SCALE_B = 64 ** -0.5
PI = float(np.pi)


def _prod(s):
    r = 1
    for v in s:
        r *= v
    return r


class Tok:
    __slots__ = ("lw", "rs", "excl")

    def __init__(self, excl=False):
        self.lw = None
        self.rs = []
        self.excl = excl


class Bld:
    ROLL = 12000

    def __init__(self, nc, es):
        self.nc = nc
        self.es = es
        self.sems = []
        self.eng = {"pe": nc.tensor, "act": nc.scalar, "dve": nc.vector, "pool": nc.gpsimd, "sp": nc.sync}
        self.esem = {}
        self.ecnt = {}
        self.pe_sems = set()
        self.waited = {e: {} for e in self.eng}
        for e in self.eng:
            self._roll(e)
        self.dq = {}
        self.semval = {}
        for q, n in (("sp", 16), ("pool", 12), ("act", 4)):
            ids = [self._new_sem(f"d_{q}{i}") for i in range(n)]
            self.dq[q] = [ids, 0]
            for i in ids:
                self.semval[i] = 0
        self.nins = 0

    def _new_sem(self, name):
        h = self.es.enter_context(self.nc.semaphore(name))
        self.sems.append(h)
        return len(self.sems) - 1

    def _roll(self, e):
        s = self._new_sem(f"e_{e}{len(self.sems)}")
        self.esem[e] = s
        self.ecnt[e] = 0
        if e == "pe":
            self.pe_sems.add(s)

    def _deps(self, reads, writes):
        deps = set()
        for t in reads:
            if t.lw is not None:
                deps.add(t.lw)
        for t in writes:
            if t.lw is not None:
                deps.add(t.lw)
            deps.update(t.rs)
        return deps

    def _waits(self, e, deps):
        w = self.waited[e]
        best = {}
        for s, v in deps:
            if e == "pe" and s in self.pe_sems:
                continue
            if w.get(s, 0) >= v:
                continue
            if best.get(s, 0) < v:
                best[s] = v
        for s, v in best.items():
            w[s] = v
            self.eng[e].wait_ge(self.sems[s], v)

    def _update(self, ev, reads, writes):
        for t in writes:
            t.lw = ev
            t.rs = []
        for t in reads:
            if t not in writes:
                t.rs.append(ev)

    def op(self, e, fn, reads=(), writes=()):
        ex = [t for t in reads if t.excl and t not in writes]
        if ex:
            writes = list(writes) + ex
        if self.ecnt[e] >= self.ROLL:
            self._roll(e)
        self._waits(e, self._deps(reads, writes))
        self.ecnt[e] += 1
        ev = (self.esem[e], self.ecnt[e])
        fn(self.eng[e]).then_inc(self.sems[ev[0]], 1)
        self._update(ev, reads, writes)
        self.nins += 1
        return ev

    def dma(self, q, out, in_, reads=(), writes=()):
        ids, i = self.dq[q]
        s = ids[i % len(ids)]
        self.dq[q][1] = i + 1
        deps = self._deps(reads, writes)
        prev = self.semval[s]
        if prev > 0:
            deps.add((s, prev))
        self._waits(q, deps)
        self.semval[s] = prev + 16
        assert self.semval[s] < 30000
        ev = (s, prev + 16)
        self.eng[q].dma_start(out=out, in_=in_).then_inc(self.sems[s], 16)
        self._update(ev, reads, writes)
        self.nins += 1
        return ev

    def barrier(self):
        evs = set()
        for e in self.eng:
            if self.ecnt[e] > 0:
                evs.add((self.esem[e], self.ecnt[e]))
        for s, v in self.semval.items():
            if v > 0:
                evs.add((s, v))
        for e in self.eng:
            w = self.waited[e]
            for s, v in evs:
                if w.get(s, 0) >= v:
                    continue
                w[s] = v
                self.eng[e].wait_ge(self.sems[s], v)


class Arena:
    def __init__(self, t, nelem):
        self.t = t
        self.cap = nelem
        self.off = 0

    def alloc(self, free_shape, dtype):
        n = _prod(free_shape)
        nb = n * (4 if dtype in (F32, I32) else 2)
        nel = (nb + 63) // 64 * 32
        assert self.off + nel <= self.cap, f"arena overflow {self.off}+{nel}>{self.cap}"
        ap = self.t[:, self.off:self.off + nb // 2]
        self.off += nel
        if dtype != BF16:
            ap = ap.bitcast(dtype)
        if len(free_shape) == 2:
            ap = ap.rearrange("p (a b) -> p a b", a=free_shape[0], b=free_shape[1])
        elif len(free_shape) == 3:
            ap = ap.rearrange("p (a b c) -> p a b c", a=free_shape[0], b=free_shape[1], c=free_shape[2])
        return ap

    def mark(self):
        return self.off

    def release(self, m):
        self.off = m


def bc(ap, shape):
    return ap.broadcast_to(list(shape))


class Prog:
    def __init__(self, mode, layer):
        self.mode = mode
        self.l = 0
        self.last = layer == 1
        if mode == "F":
            self.NT, self.ctx_tiles, self.ctx_keys = 34, {32, 33}, [32, 33]
        else:
            self.NT, self.ctx_tiles, self.ctx_keys = 17, {16}, [0, 1]
        self.NTOK = self.NT * 128
        nl = 2 if mode == "F" else 1
        nc = bass.Bass("TRN2", target_bir_lowering=False)
        self.nc = nc
        self.es = ExitStack()
        es = self.es
        dt = nc.dram_tensor

        def inp(name, shape, dtype=F32):
            return dt(name, list(shape), dtype, kind="ExternalInput").ap()

        def outp(name, shape, dtype=F32):
            return dt(name, list(shape), dtype, kind="ExternalOutput").ap()

        self.inp = inp
        self.outp = outp
        self.xin = inp("xin", [self.NTOK, D])
        self.xsrc = self.xin
        self.cvecT = inp("cvecT", [128, 2, 8])
        self.ident = inp("ident", [128, 128])
        self.rowpos = inp("rowpos", [128, self.NT])
        self.colpos = inp("colpos", [128, self.NT])
        nseg = 2 if mode == "A" else 6
        self.w_mod = inp("w_mod", [nl, D, nseg * D])
        self.b_mod = inp("b_mod", [nl, nseg * D])
        self.g_attn = inp("g_attn", [nl, D])
        self.w_in = inp("w_in", [nl, D, IN_COLS])
        if mode in ("A", "F"):
            self.g_ckv = inp("g_ckv", [nl, 128])
            self.w_ukv = inp("w_ukv", [nl, 128, 1024])
            self.g_ka = inp("g_ka", [nl, 96])
            self.g_kb = inp("g_kb", [nl, 64])
        if mode == "A":
            self.KaT = outp("KaT", [8, 96, NTOK], BF16)
            self.KbT = outp("KbT", [2, 64, NTOK], BF16)
            self.Va = outp("Va", [NTOK, 8, 64], BF16)
            self.Vb = outp("Vb", [NTOK, 2, 64], BF16)
        if mode in ("B", "F"):
            self.g_ffn = inp("g_ffn", [nl, D])
            self.g_cq = inp("g_cq", [nl, 256])
            self.w_uq = inp("w_uq", [nl, 256, 768])
            self.g_qa = inp("g_qa", [nl, 96])
            self.g_qb = inp("g_qb", [nl, 64])
            self.w_oa = inp("w_oa", [nl, 512, D])
            self.w_ob = inp("w_ob", [nl, 512, D])
            self.w_out = inp("w_out", [nl, D, D])
            self.w_router = inp("w_router", [D, 16])
            self.b_router = inp("b_router", [1, 16])
            self.w_e_gate = inp("w_e_gate", [nl, 16, D, 512])
            self.w_e_up = inp("w_e_up", [nl, 16, D, 512])
            self.w_e_down = inp("w_e_down", [nl, 16, 512, D])
            self.w_s_gate = inp("w_s_gate", [nl, D, 512])
            self.w_s_up = inp("w_s_up", [nl, D, 512])
            self.w_s_down = inp("w_s_down", [nl, 512, D])
            if mode == "B":
                self.KaT = inp("KaT", [8, 96, NKEY], BF16)
                self.KbT = inp("KbT", [2, 64, NKEY], BF16)
                self.Va = inp("Va", [NKEY, 8, 64], BF16)
                self.Vb = inp("Vb", [NKEY, 2, 64], BF16)
                self.xout = outp("xout", [NTOK, D])
            else:
                self.KaT = dt("KaT", [8, 96, NKEY], BF16).ap()
                self.KbT = dt("KbT", [2, 64, NKEY], BF16).ap()
                self.Va = dt("Va", [128, NKT, 8 * 66], BF16).ap()
                self.Vb = dt("Vb", [128, NKT, 2 * 66], BF16).ap()
                self.xs1 = dt("xs1", [NKEY, D], F32).ap()
                self.xout = outp("xout", [2048, D])
            self.QaT = dt("QaT", [8, 96, self.NTOK], BF16).ap()
            self.QbT = dt("QbT", [8, 64, self.NTOK], BF16).ap()
            self.Gs = dt("Gs", [self.NTOK, 2048], BF16).ap()
            self.OT = dt("OT", [16, 64, self.NTOK], BF16).ap()
        arena_t = es.enter_context(nc.sbuf_tensor("arena", [128, 106000], BF16))
        self.ar = Arena(arena_t, 106000)
        psum_t = es.enter_context(nc.psum_tensor("psum", [128, 4096], F32))
        self.psum = psum_t
        self.b = Bld(nc, es)
        self.ptok = [Tok(excl=True) for _ in range(8)]

    def bank(self, i, n=1):
        return self.psum[:, i * 512:(i + n) * 512]

    def bank_bf(self, i):
        return self.psum[:, i * 512:(i + 1) * 512].bitcast(BF16)

    def load_bcast(self, dst, src_row):
        t = Tok()
        self.b.dma("sp", dst, src_row.partition_broadcast(128), writes=[t])
        return t

    def setup_consts(self):
        b, ar = self.b, self.ar
        self.identf = ar.alloc([128], F32)
        self.identb = ar.alloc([128], BF16)
        self.t_identf = Tok()
        self.t_identb = Tok()
        b.dma("sp", self.identf, self.ident, writes=[self.t_identf])
        b.dma("pool", self.identb, self.ident, writes=[self.t_identb])
        self.onesf = ar.alloc([64], F32)
        self.t_ones = Tok()
        b.op("dve", lambda e: e.memset(self.onesf, 1.0), writes=[self.t_ones])

    def setup_rope(self):
        b, ar = self.b, self.ar
        NT = self.NT
        rp = ar.alloc([NT], F32)
        cp = ar.alloc([NT], F32)
        t_pos = Tok()
        b.dma("sp", rp, self.rowpos, writes=[t_pos])
        b.dma("sp", cp, self.colpos, writes=[t_pos])
        self.rope = {}
        for name, half in (("a", 8), ("b", 16)):
            R = 4 * half
            C = ar.alloc([NT, R], F32)
            S = ar.alloc([NT, R], F32)
            tC = Tok()
            m = ar.mark()
            inv = ar.alloc([half], F32)
            t_inv = Tok()
            for j in range(half):
                val = float(THETA ** (-(2.0 * j) / (2 * half)))
                b.op("dve", lambda e, j=j, val=val: e.memset(inv[:, j:j + 1], val), writes=[t_inv])
            ang = ar.alloc([NT, half], F32)
            tmpf = ar.alloc([NT, half], F32)
            tmpi = ar.alloc([NT, half], I32)
            red = ar.alloc([NT, half], F32)
            t_ang, t_f, t_i, t_r = Tok(), Tok(), Tok(), Tok()
            for ci, pos in enumerate((rp, cp)):
                b.op("dve", lambda e, pos=pos: e.tensor_tensor(
                    out=ang, in0=bc(pos.unsqueeze(2), [128, NT, half]), in1=bc(inv.unsqueeze(1), [128, NT, half]),
                    op=ALU.mult), reads=[t_pos, t_inv], writes=[t_ang])
                for which, shift in (("sin", 0.0), ("cos", PI / 2)):
                    b.op("dve", lambda e, shift=shift: e.tensor_scalar(
                        out=tmpf, in0=ang, scalar1=shift, scalar2=1.0 / (2 * PI), op0=ALU.add, op1=ALU.mult),
                        reads=[t_ang], writes=[t_f])
                    b.op("dve", lambda e: e.tensor_copy(out=tmpi, in_=tmpf), reads=[t_f], writes=[t_i])
                    b.op("dve", lambda e: e.tensor_copy(out=tmpf, in_=tmpi), reads=[t_i], writes=[t_f])
                    b.op("dve", lambda e: e.scalar_tensor_tensor(
                        out=red, in0=tmpf, scalar=-2 * PI, in1=ang, op0=ALU.mult, op1=ALU.add),
                        reads=[t_f, t_ang], writes=[t_r])
                    b.op("dve", lambda e, shift=shift: e.tensor_scalar(
                        out=red, in0=red, scalar1=shift, scalar2=PI, op0=ALU.add, op1=ALU.min),
                        reads=[t_r], writes=[t_r])
                    b.op("dve", lambda e: e.tensor_scalar(
                        out=red, in0=red, scalar1=-PI, scalar2=None, op0=ALU.max), reads=[t_r], writes=[t_r])
                    base = ci * 2 * half
                    if which == "sin":
                        b.op("act", lambda e, base=base: e.activation(
                            out=S[:, :, base + half:base + 2 * half], in_=red, func=AF.Sin), reads=[t_r], writes=[tC])
                        b.op("dve", lambda e, base=base: e.tensor_scalar(
                            out=S[:, :, base:base + half], in0=S[:, :, base + half:base + 2 * half], scalar1=-1.0,
                            scalar2=None, op0=ALU.mult), reads=[tC], writes=[tC])
                    else:
                        b.op("act", lambda e, base=base: e.activation(
                            out=C[:, :, base:base + half], in_=red, func=AF.Sin), reads=[t_r], writes=[tC])
                        b.op("dve", lambda e, base=base: e.tensor_copy(
                            out=C[:, :, base + half:base + 2 * half], in_=C[:, :, base:base + half]),
                            reads=[tC], writes=[tC])
            self.b.barrier()
            ar.release(m)
            self.rope[name] = (C, S, tC, half)

    def emit_mod(self, segs):
        b, ar, l = self.b, self.ar, self.l
        res = {}
        for s in (0, 1):
            for seg in segs:
                res[(s, seg)] = (ar.alloc([D], F32), Tok())
        m = ar.mark()
        cT = ar.alloc([2, 8], F32)
        sc = ar.alloc([2, 8], F32)
        scb = ar.alloc([2, 8, 128], BF16)
        t_c, t_sc, t_scb = Tok(), Tok(), Tok()
        b.dma("sp", cT, self.cvecT, writes=[t_c])
        b.op("act", lambda e: e.activation(out=sc, in_=cT, func=AF.Silu), reads=[t_c], writes=[t_sc])
        for s in (0, 1):
            b.op("dve", lambda e, s=s: e.tensor_copy(out=scb[:, s], in_=bc(sc[:, s].unsqueeze(2), [128, 8, 128])),
                 reads=[t_sc], writes=[t_scb])
        wm = [ar.alloc([8, 512], BF16) for _ in range(2)]
        t_wm = [Tok(), Tok()]
        bm = [ar.alloc([512], F32) for _ in range(2)]
        t_bm = [Tok(), Tok()]
        k = 0
        for seg in segs:
            for j in range(2):
                c0 = seg * D + j * 512
                buf = k % 2
                k += 1
                b.dma("pool", wm[buf], self.w_mod[l, :, c0:c0 + 512].rearrange("(k p) c -> p k c", p=128),
                      writes=[t_wm[buf]])
                b.dma("sp", bm[buf], self.b_mod[l:l + 1, c0:c0 + 512].partition_broadcast(128), writes=[t_bm[buf]])
                for s in (0, 1):
                    pb = 2 * buf + s
                    for kk in range(8):
                        b.op("pe", lambda e, kk=kk, s=s, buf=buf, pb=pb: e.matmul(
                            self.bank(pb), lhsT=scb[:, s, kk, :], rhs=wm[buf][:, kk, :], start=(kk == 0), stop=(kk == 7)),
                            reads=[t_scb, t_wm[buf]], writes=[self.ptok[pb]])
                    dst, tk = res[(s, seg)]
                    b.op("dve", lambda e, dst=dst, j=j, pb=pb, buf=buf: e.tensor_tensor(
                        out=dst[:, j * 512:(j + 1) * 512], in0=self.bank(pb), in1=bm[buf], op=ALU.add),
                        reads=[self.ptok[pb], t_bm[buf]], writes=[tk])
        b.barrier()
        ar.release(m)
        return res

    def run_il(self, gens, depth=2):
        it = iter(gens)
        active = []
        while True:
            while len(active) < depth:
                g = next(it, None)
                if g is None:
                    break
                active.append(g)
            if not active:
                break
            for g in list(active):
                try:
                    next(g)
                except StopIteration:
                    active.remove(g)

    def rstd_from_ss(self, ss, n_h, denom, t_ss):
        b = self.b
        b.op("dve", lambda e: e.tensor_scalar(out=ss, in0=ss, scalar1=1.0 / denom, scalar2=EPS, op0=ALU.mult,
                                              op1=ALU.add), reads=[t_ss], writes=[t_ss])
        yield
        b.op("act", lambda e: e.activation(out=ss, in_=ss, func=AF.Sqrt), reads=[t_ss], writes=[t_ss])
        yield
        b.op("dve", lambda e: e.reciprocal(out=ss, in_=ss), reads=[t_ss], writes=[t_ss])
        yield

    def norm_mod_tile(self, xt, t_x, gm, sh, hout, t_h, scratch, t_scr, st, t_st):
        b = self.b
        b.op("act", lambda e: e.activation(out=scratch, in_=xt, func=AF.Square, accum_out=st),
             reads=[t_x], writes=[t_scr, t_st])
        yield
        yield from self.rstd_from_ss(st, 1, D, t_st)
        b.op("dve", lambda e: e.scalar_tensor_tensor(out=scratch, in0=xt, scalar=st, in1=gm[0], op0=ALU.mult,
                                                     op1=ALU.mult), reads=[t_x, t_st, gm[1]], writes=[t_scr])
        yield
        b.op("dve", lambda e: e.tensor_tensor(out=hout, in0=scratch, in1=sh[0], op=ALU.add),
             reads=[t_scr, sh[1]], writes=[t_h])
        yield

    def transpose_rows(self, src, t_src, nchunk, csize, pbank, dst, t_dst, ident=None, evac="act"):
        b = self.b
        pv = self.bank_bf(pbank)
        for c in range(nchunk):
            b.op("pe", lambda e, c=c: e.transpose(pv[0:csize, c * 128:(c + 1) * 128], src[:, c, :], self.identb),
                 reads=[t_src, self.t_identb], writes=[self.ptok[pbank]])
        yield
        srcv = pv[0:csize, 0:nchunk * 128].rearrange("p (a b) -> p a b", a=nchunk, b=128)
        if evac == "act":
            b.op("act", lambda e: e.activation(out=dst, in_=srcv, func=AF.Copy), reads=[self.ptok[pbank]], writes=[t_dst])
        else:
            b.op("dve", lambda e: e.tensor_copy(out=dst, in_=srcv), reads=[self.ptok[pbank]], writes=[t_dst])
        yield

    def head_norm(self, src, t_src, H, Dh, extra_ss, gvec, t_g, sq, t_sq, ss, t_ss, dst_f, t_dstf):
        b = self.b
        b.op("act", lambda e: e.activation(out=sq, in_=src, func=AF.Square), reads=[t_src], writes=[t_sq])
        yield
        b.op("dve", lambda e: e.tensor_reduce(out=ss, in_=sq, axis=AX.X, op=ALU.add), reads=[t_sq], writes=[t_ss])
        yield
        denom = Dh
        if extra_ss is not None:
            ex, t_ex, n_ex = extra_ss
            b.op("dve", lambda e: e.tensor_tensor(out=ss, in0=ss, in1=bc(ex, [128, H]), op=ALU.add),
                 reads=[t_ss, t_ex], writes=[t_ss])
            yield
            denom = Dh + n_ex
        yield from self.rstd_from_ss(ss, H, denom, t_ss)
        b.op("dve", lambda e: e.tensor_tensor(out=dst_f, in0=src, in1=bc(ss.unsqueeze(2), [128, H, Dh]), op=ALU.mult),
             reads=[t_src, t_ss], writes=[t_dstf])
        yield
        b.op("dve", lambda e: e.tensor_tensor(out=dst_f, in0=dst_f, in1=bc(gvec.unsqueeze(1), [128, H, Dh]),
                                              op=ALU.mult), reads=[t_dstf, t_g], writes=[t_dstf])
        yield

    def rope_apply(self, v, t_v, H, which, tile, dst, t_dst, t1, t2, t_t):
        b = self.b
        C, S, tC, half = self.rope[which]
        R = 4 * half
        Ct = C[:, tile, :]
        St = S[:, tile, :]
        b.op("dve", lambda e: e.tensor_tensor(out=t1, in0=v, in1=bc(Ct.unsqueeze(1), [128, H, R]), op=ALU.mult),
             reads=[t_v, tC], writes=[t_t])
        v4 = v.rearrange("p h (c t j) -> p h c t j", c=2, t=2, j=half)
        t24 = t2.rearrange("p h (c t j) -> p h c t j", c=2, t=2, j=half)
        S4 = St.rearrange("p (c t j) -> p c t j", c=2, t=2, j=half)
        for tt in range(2):
            b.op("dve", lambda e, tt=tt: e.tensor_tensor(
                out=t24[:, :, :, tt, :], in0=v4[:, :, :, 1 - tt, :],
                in1=bc(S4[:, :, tt, :].unsqueeze(1), [128, H, 2, half]), op=ALU.mult),
                reads=[t_v, tC], writes=[t_t])
        yield
        b.op("dve", lambda e: e.tensor_tensor(out=dst, in0=t1, in1=t2, op=ALU.add), reads=[t_t], writes=[t_dst])
        yield

    def _common_setup(self):
        b, ar, l = self.b, self.ar, self.l
        self.setup_rope()
        mod = self.emit_mod([0, 1])
        gat = ar.alloc([D], F32)
        t_gat = self.load_bcast(gat, self.g_attn[l:l + 1, :])
        gm = {}
        for s in (0, 1):
            ap, tk = mod[(s, 1)]
            b.op("dve", lambda e, ap=ap: e.scalar_tensor_tensor(out=ap, in0=ap, scalar=1.0, in1=gat, op0=ALU.add,
                                                                op1=ALU.mult), reads=[tk, t_gat], writes=[tk])
            gm[s] = (ap, tk)
        return mod, gm

    def _q_chain(self, u, ti, pb, W):
        b = self.b
        PB_CQ, PB_QB, PB_CT, PB_QA, PB_QTA, PB_QTB = pb + 1, pb + 2, pb, pb + 2, pb + 2, pb + 3
        for kk in range(8):
            b.op("pe", lambda e, kk=kk: e.matmul(self.bank(PB_CQ)[:, 0:256], lhsT=u.hT[:, kk, :], rhs=W.wq1[:, kk, :],
                                                 start=(kk == 0), stop=(kk == 7)),
                 reads=[u.t_hT, W.t_w], writes=[self.ptok[PB_CQ]])
        for kk in range(8):
            b.op("pe", lambda e, kk=kk: e.matmul(self.bank(PB_QB), lhsT=u.hT[:, kk, :], rhs=W.wq2[:, kk, :],
                                                 start=(kk == 0), stop=(kk == 7)),
                 reads=[u.t_hT, W.t_w], writes=[self.ptok[PB_QB]])
        yield
        cq = self.bank(PB_CQ)[:, 0:256]
        b.op("act", lambda e: e.activation(out=u.junk, in_=cq, func=AF.Square, accum_out=u.st[:, 1:2]),
             reads=[self.ptok[PB_CQ]], writes=[u.t_junk, u.t_st])
        qbp = self.bank(PB_QB).rearrange("p (h d) -> p h d", h=8, d=64)
        b.op("act", lambda e: e.activation(out=u.qbf, in_=qbp, func=AF.Copy), reads=[self.ptok[PB_QB]],
             writes=[u.t_qbf])
        yield
        yield from self.rstd_from_ss(u.st[:, 1:2], 1, 256, u.t_st)
        b.op("dve", lambda e: e.scalar_tensor_tensor(out=u.cqn.rearrange("p a b -> p (a b)"), in0=cq,
                                                     scalar=u.st[:, 1:2], in1=W.gcq, op0=ALU.mult, op1=ALU.mult),
             reads=[self.ptok[PB_CQ], u.t_st, W.t_g], writes=[u.t_cqn])
        yield
        yield from self.transpose_rows(u.cqn, u.t_cqn, 2, 128, PB_CT, u.cqnT, u.t_cqnT, evac="dve")
        for j, (c0, c1) in enumerate(((0, 512), (512, 768))):
            for kk in range(2):
                b.op("pe", lambda e, j=j, kk=kk, c0=c0, c1=c1: e.matmul(
                    self.bank(PB_QA + j)[:, 0:c1 - c0], lhsT=u.cqnT[:, kk, :], rhs=W.wuq[:, kk, c0:c1],
                    start=(kk == 0), stop=(kk == 1)), reads=[u.t_cqnT, W.t_w], writes=[self.ptok[PB_QA + j]])
        for j in range(4):
            pbg = PB_CQ if j % 2 == 0 else PB_CT
            for kk in range(8):
                b.op("pe", lambda e, kk=kk, j=j, pb=pbg: e.matmul(self.bank(pb), lhsT=u.hT[:, kk, :],
                                                                 rhs=W.wg[:, kk, j * 512:(j + 1) * 512],
                                                                 start=(kk == 0), stop=(kk == 7)),
                     reads=[u.t_hT, W.t_w], writes=[self.ptok[pbg]])
            b.op("act", lambda e, j=j, pb=pbg: e.activation(out=u.Gsb[:, j * 512:(j + 1) * 512], in_=self.bank(pb),
                                                            func=AF.Sigmoid), reads=[self.ptok[pbg]], writes=[u.t_Gsb])
            if j == 0:
                qa = self.bank(PB_QA, 2)[:, 0:768].rearrange("p (h d) -> p h d", h=8, d=96)
                b.op("act", lambda e: e.activation(out=u.qaf, in_=qa, func=AF.Copy),
                     reads=[self.ptok[PB_QA], self.ptok[PB_QA + 1]], writes=[u.t_qaf])
            yield
        b.dma("sp", self.Gs[ti * 128:(ti + 1) * 128, :], u.Gsb, reads=[u.t_Gsb], writes=[W.t_out])
        yield from self.head_norm(u.qaf, u.t_qaf, 8, 96, None, W.gqa, W.t_g2, u.sq, u.t_sq, u.ss8, u.t_ss8, u.qaf, u.t_qaf)
        b.op("act", lambda e: e.activation(out=u.Qab[:, :, 0:64], in_=u.qaf[:, :, 0:64], func=AF.Copy),
             reads=[u.t_qaf], writes=[u.t_Qab])
        yield from self.rope_apply(u.qaf[:, :, 64:96], u.t_qaf, 8, "a", ti, u.Qab[:, :, 64:96], u.t_Qab,
                                   u.r1[:, :, 0:32], u.r2[:, :, 0:32], u.t_r1)
        yield from self.transpose_rows(u.Qab, u.t_Qab, 8, 96, PB_QTA, u.QaTs[0:96], u.t_QaTs)
        b.dma("sp", self.QaT[:, :, ti * 128:(ti + 1) * 128].rearrange("h d t -> d h t"), u.QaTs[0:96],
              reads=[u.t_QaTs], writes=[W.t_out])
        yield from self.head_norm(u.qbf, u.t_qbf, 8, 64, None, W.gqb, W.t_g3, u.sq[:, :, 0:64], u.t_sq, u.ss8b, u.t_ss8b,
                                  u.qbf, u.t_qbf)
        yield from self.rope_apply(u.qbf, u.t_qbf, 8, "b", ti, u.Qbb, u.t_Qbb, u.r1, u.r2, u.t_r1)
        yield from self.transpose_rows(u.Qbb, u.t_Qbb, 8, 64, PB_QTB, u.QbTs[0:64], u.t_QbTs, evac="dve")
        b.dma("sp", self.QbT[:, :, ti * 128:(ti + 1) * 128].rearrange("h d t -> d h t"), u.QbTs[0:64],
              reads=[u.t_QbTs], writes=[W.t_out])
        yield

    def phase_kvq(self, tiles, qtiles, setup):
        from types import SimpleNamespace as NS
        b, ar, l = self.b, self.ar, self.l
        m0 = ar.mark()
        mod, gm = setup
        wkv1 = ar.alloc([8, 160], BF16)
        wkv2 = ar.alloc([8, 256], BF16)
        wukv = ar.alloc([1024], BF16)
        t_w = Tok()
        wv = self.w_in[l].rearrange("(k p) c -> p k c", p=128)
        b.dma("pool", wkv1, wv[:, :, C_CKV:C_QB], writes=[t_w])
        b.dma("pool", wkv2, wv[:, :, C_KB:C_GA], writes=[t_w])
        b.dma("pool", wukv, self.w_ukv[l], writes=[t_w])
        gckv = ar.alloc([128], F32)
        gka = ar.alloc([96], F32)
        gkb = ar.alloc([64], F32)
        t_g = self.load_bcast(gckv, self.g_ckv[l:l + 1, :])
        t_g2 = self.load_bcast(gka, self.g_ka[l:l + 1, :])
        t_g3 = self.load_bcast(gkb, self.g_kb[l:l + 1, :])

        W = NS()
        W.wq1 = ar.alloc([8, 256], BF16)
        W.wq2 = ar.alloc([8, 512], BF16)
        W.wg = ar.alloc([8, 2048], BF16)
        W.wuq = ar.alloc([2, 768], BF16)
        W.t_w = Tok()
        b.dma("pool", W.wq1, wv[:, :, C_CQ:C_CKV], writes=[W.t_w])
        b.dma("pool", W.wq2, wv[:, :, C_QB:C_KB], writes=[W.t_w])
        for j in range(4):
            b.dma("pool", W.wg[:, :, j * 512:(j + 1) * 512], wv[:, :, C_GA + j * 512:C_GA + (j + 1) * 512],
                  writes=[W.t_w])
        b.dma("pool", W.wuq, self.w_uq[l].rearrange("(k p) c -> p k c", p=128), writes=[W.t_w])
        W.gcq = ar.alloc([256], F32)
        W.gqa = ar.alloc([96], F32)
        W.gqb = ar.alloc([64], F32)
        W.t_g = self.load_bcast(W.gcq, self.g_cq[l:l + 1, :])
        W.t_g2 = self.load_bcast(W.gqa, self.g_qa[l:l + 1, :])
        W.t_g3 = self.load_bcast(W.gqb, self.g_qb[l:l + 1, :])
        W.t_out = Tok()
        qset = set(qtiles)

        def mk():
            u = NS()
            for name, shp, dt_ in (("cqn", [2, 128], BF16), ("cqnT", [2, 128], BF16), ("ss8b", [8], F32),
                                   ("qaf", [8, 96], F32), ("qbf", [8, 64], F32), ("Qab", [8, 96], BF16),
                                   ("Qbb", [8, 64], BF16), ("QaTs", [8, 128], BF16), ("QbTs", [8, 128], BF16),
                                   ("Gsb", [2048], BF16), ("scr", [D], F32), ("st", [8], F32), ("h", [8, 128], BF16),
                                   ("hT", [8, 128], BF16), ("ckvn", [1, 128], BF16), ("ckvnT", [1, 128], BF16),
                                   ("ss8", [8], F32), ("ss2", [2], F32), ("sq", [8, 96], F32), ("kaf", [8, 64], F32),
                                   ("krf", [1, 32], F32), ("krr", [1, 32], F32), ("r1", [8, 64], F32),
                                   ("r2", [8, 64], F32), ("Kab", [8, 96], BF16), ("KaTs", [8, 128], BF16),
                                   ("Vab", [8, 66], BF16), ("kbf", [2, 64], F32), ("Kbb", [2, 64], BF16),
                                   ("KbTs", [2, 128], BF16), ("Vbb", [2, 66], BF16), ("sskr", [1], F32),
                                   ("junk", [256], F32)):
                setattr(u, name, ar.alloc(shp, dt_))
                setattr(u, "t_" + name, Tok())
            u.sq64, u.t_sq64 = u.sq[:, :, 0:64], u.t_sq
            u.junk128, u.t_junk128 = u.junk[:, 0:128], u.t_junk
            return u
        sets = [mk(), mk()]
        for u_ in sets:
            b.op("dve", lambda e, u_=u_: e.memset(u_.Vab[:, :, 64:66], 1.0), writes=[u_.t_Vab])
            b.op("dve", lambda e, u_=u_: e.memset(u_.Vbb[:, :, 64:66], 1.0), writes=[u_.t_Vbb])
        t_out = Tok()
        xts = [ar.alloc([D], F32) for _ in range(4)]
        t_xts = [Tok() for _ in range(4)]

        def issue(j):
            if j < len(tiles):
                tj = tiles[j]
                b.dma("sp", xts[j % 4], self.xsrc[tj * 128:(tj + 1) * 128, :], writes=[t_xts[j % 4]])
        issue(0)
        issue(1)

        def tile(idx, ti):
            u = sets[idx % 2]
            u.xt, u.t_xt = xts[idx % 4], t_xts[idx % 4]
            issue(idx + 2)
            pb = 4 * (idx % 2)
            PB_HT, PB_KV1, PB_KV2, PB_CT, PB_KVA, PB_KAT, PB_KBT = pb, pb + 1, pb + 2, pb, pb + 2, pb, pb + 1
            s = 1 if ti in self.ctx_tiles else 0
            yield from self.norm_mod_tile(u.xt, u.t_xt, gm[s], mod[(s, 0)], u.h.rearrange("p a b -> p (a b)"), u.t_h,
                                          u.scr, u.t_scr, u.st[:, 0:1], u.t_st)
            yield from self.transpose_rows(u.h, u.t_h, 8, 128, PB_HT, u.hT, u.t_hT)
            for kk in range(8):
                b.op("pe", lambda e, kk=kk: e.matmul(self.bank(PB_KV1)[:, 0:160], lhsT=u.hT[:, kk, :], rhs=wkv1[:, kk, :],
                                                     start=(kk == 0), stop=(kk == 7)),
                     reads=[u.t_hT, t_w], writes=[self.ptok[PB_KV1]])
            for kk in range(8):
                b.op("pe", lambda e, kk=kk: e.matmul(self.bank(PB_KV2)[:, 0:256], lhsT=u.hT[:, kk, :], rhs=wkv2[:, kk, :],
                                                     start=(kk == 0), stop=(kk == 7)),
                     reads=[u.t_hT, t_w], writes=[self.ptok[PB_KV2]])
            yield
            ckv = self.bank(PB_KV1)[:, 0:128]
            kr = self.bank(PB_KV1)[:, 128:160]
            b.op("act", lambda e: e.activation(out=u.junk128, in_=ckv, func=AF.Square, accum_out=u.st[:, 1:2]),
                 reads=[self.ptok[PB_KV1]], writes=[u.t_junk, u.t_st])
            yield
            yield from self.rstd_from_ss(u.st[:, 1:2], 1, 128, u.t_st)
            b.op("dve", lambda e: e.scalar_tensor_tensor(out=u.ckvn[:, 0, :], in0=ckv, scalar=u.st[:, 1:2], in1=gckv,
                                                         op0=ALU.mult, op1=ALU.mult),
                 reads=[self.ptok[PB_KV1], u.t_st, t_g], writes=[u.t_ckvn])
            yield
            kbp = self.bank(PB_KV2)[:, 0:128].rearrange("p (h d) -> p h d", h=2, d=64)
            vbp = self.bank(PB_KV2)[:, 128:256].rearrange("p (h d) -> p h d", h=2, d=64)
            b.op("act", lambda e: e.activation(out=u.Vbb[:, :, 0:64], in_=vbp, func=AF.Copy), reads=[self.ptok[PB_KV2]],
                 writes=[u.t_Vbb])
            b.op("act", lambda e: e.activation(out=u.kbf, in_=kbp, func=AF.Copy), reads=[self.ptok[PB_KV2]],
                 writes=[u.t_kbf])
            yield
            b.dma("sp", self.Vb[:, ti, :], u.Vbb.rearrange("p a b -> p (a b)"), reads=[u.t_Vbb], writes=[t_out])
            b.op("act", lambda e: e.activation(out=u.junk128[:, 0:32], in_=kr, func=AF.Square, accum_out=u.sskr),
                 reads=[self.ptok[PB_KV1]], writes=[u.t_junk, u.t_sskr])
            b.op("dve", lambda e: e.tensor_tensor(out=u.krf[:, 0, :], in0=kr, in1=gka[:, 64:96], op=ALU.mult),
                 reads=[self.ptok[PB_KV1], t_g2], writes=[u.t_krf])
            yield
            yield from self.transpose_rows(u.ckvn, u.t_ckvn, 1, 128, PB_CT, u.ckvnT, u.t_ckvnT, evac="dve")
            for j in range(2):
                b.op("pe", lambda e, j=j: e.matmul(self.bank(PB_KVA + j), lhsT=u.ckvnT[:, 0, :],
                                                   rhs=wukv[:, j * 512:(j + 1) * 512], start=True, stop=True),
                     reads=[u.t_ckvnT, t_w], writes=[self.ptok[PB_KVA + j]])
            yield
            kva = self.bank(PB_KVA, 2).rearrange("p (h d) -> p h d", h=8, d=128)
            t_kva = [self.ptok[PB_KVA], self.ptok[PB_KVA + 1]]
            knope = kva[:, :, 0:64]
            yield from self.rope_apply(u.krf, u.t_krf, 1, "a", ti, u.krr, u.t_krr, u.r1[:, 0:1, 0:32], u.r2[:, 0:1, 0:32],
                                       u.t_r1)
            b.op("act", lambda e: e.activation(out=u.kaf, in_=knope, func=AF.Copy), reads=t_kva, writes=[u.t_kaf])
            b.op("act", lambda e: e.activation(out=u.Vab[:, :, 0:64], in_=kva[:, :, 64:128], func=AF.Copy), reads=t_kva,
                 writes=[u.t_Vab])
            yield
            b.dma("sp", self.Va[:, ti, :], u.Vab.rearrange("p a b -> p (a b)"), reads=[u.t_Vab], writes=[t_out])
            b.op("act", lambda e: e.activation(out=u.sq64, in_=u.kaf, func=AF.Square), reads=[u.t_kaf], writes=[u.t_sq])
            yield
            b.op("dve", lambda e: e.tensor_reduce(out=u.ss8, in_=u.sq64, axis=AX.X, op=ALU.add), reads=[u.t_sq],
                 writes=[u.t_ss8])
            yield
            b.op("dve", lambda e: e.tensor_tensor(out=u.ss8, in0=u.ss8, in1=bc(u.sskr, [128, 8]), op=ALU.add),
                 reads=[u.t_ss8, u.t_sskr], writes=[u.t_ss8])
            yield
            yield from self.rstd_from_ss(u.ss8, 8, 96, u.t_ss8)
            b.op("dve", lambda e: e.tensor_tensor(out=u.kaf, in0=u.kaf, in1=bc(u.ss8.unsqueeze(2), [128, 8, 64]),
                                                  op=ALU.mult), reads=[u.t_kaf, u.t_ss8], writes=[u.t_kaf])
            yield
            b.op("dve", lambda e: e.tensor_tensor(out=u.Kab[:, :, 0:64], in0=u.kaf,
                                                  in1=bc(gka[:, 0:64].unsqueeze(1), [128, 8, 64]), op=ALU.mult),
                 reads=[u.t_kaf, t_g2], writes=[u.t_Kab])
            b.op("dve", lambda e: e.tensor_tensor(out=u.Kab[:, :, 64:96], in0=bc(u.krr, [128, 8, 32]),
                                                  in1=bc(u.ss8.unsqueeze(2), [128, 8, 32]), op=ALU.mult),
                 reads=[u.t_krr, u.t_ss8], writes=[u.t_Kab])
            yield
            yield from self.transpose_rows(u.Kab, u.t_Kab, 8, 96, PB_KAT, u.KaTs[0:96], u.t_KaTs)
            b.dma("sp", self.KaT[:, :, ti * 128:(ti + 1) * 128].rearrange("h d t -> d h t"), u.KaTs[0:96],
                  reads=[u.t_KaTs], writes=[t_out])
            yield from self.head_norm(u.kbf, u.t_kbf, 2, 64, None, gkb, t_g3, u.sq64[:, 0:2, :], u.t_sq, u.ss2, u.t_ss2,
                                      u.kbf, u.t_kbf)
            yield from self.rope_apply(u.kbf, u.t_kbf, 2, "b", ti, u.Kbb, u.t_Kbb, u.r1[:, 0:2, :], u.r2[:, 0:2, :], u.t_r1)
            yield from self.transpose_rows(u.Kbb, u.t_Kbb, 2, 64, PB_KBT, u.KbTs[0:64], u.t_KbTs, evac="dve")
            b.dma("sp", self.KbT[:, :, ti * 128:(ti + 1) * 128].rearrange("h d t -> d h t"), u.KbTs[0:64],
                  reads=[u.t_KbTs], writes=[t_out])
            yield
            if ti in qset:
                yield from self._q_chain(u, ti, pb, W)
        self.run_il((tile(i, t) for i, t in enumerate(tiles)), depth=2)
        b.barrier()
        ar.release(m0)

    def phase_kv(self, tiles, setup):
        from types import SimpleNamespace as NS
        b, ar, l = self.b, self.ar, self.l
        m0 = ar.mark()
        mod, gm = setup
        wkv1 = ar.alloc([8, 160], BF16)
        wkv2 = ar.alloc([8, 256], BF16)
        wukv = ar.alloc([1024], BF16)
        t_w = Tok()
        wv = self.w_in[l].rearrange("(k p) c -> p k c", p=128)
        b.dma("pool", wkv1, wv[:, :, C_CKV:C_QB], writes=[t_w])
        b.dma("pool", wkv2, wv[:, :, C_KB:C_GA], writes=[t_w])
        b.dma("pool", wukv, self.w_ukv[l], writes=[t_w])
        gckv = ar.alloc([128], F32)
        gka = ar.alloc([96], F32)
        gkb = ar.alloc([64], F32)
        t_g = self.load_bcast(gckv, self.g_ckv[l:l + 1, :])
        t_g2 = self.load_bcast(gka, self.g_ka[l:l + 1, :])
        t_g3 = self.load_bcast(gkb, self.g_kb[l:l + 1, :])

        def mk():
            u = NS()
            for name, shp, dt_ in (("scr", [D], F32), ("st", [8], F32), ("h", [8, 128], BF16),
                                   ("hT", [8, 128], BF16), ("ckvn", [1, 128], BF16), ("ckvnT", [1, 128], BF16),
                                   ("ss8", [8], F32), ("ss2", [2], F32), ("sq", [8, 64], F32), ("kaf", [8, 64], F32),
                                   ("krf", [1, 32], F32), ("krr", [1, 32], F32), ("r1", [8, 64], F32),
                                   ("r2", [8, 64], F32), ("Kab", [8, 96], BF16), ("KaTs", [8, 128], BF16),
                                   ("Vab", [8, 66], BF16), ("kbf", [2, 64], F32), ("Kbb", [2, 64], BF16),
                                   ("KbTs", [2, 128], BF16), ("Vbb", [2, 66], BF16), ("sskr", [1], F32),
                                   ("junk", [128], F32)):
                setattr(u, name, ar.alloc(shp, dt_))
                setattr(u, "t_" + name, Tok())
            return u
        sets = [mk(), mk()]
        for u_ in sets:
            b.op("dve", lambda e, u_=u_: e.memset(u_.Vab[:, :, 64:66], 1.0), writes=[u_.t_Vab])
            b.op("dve", lambda e, u_=u_: e.memset(u_.Vbb[:, :, 64:66], 1.0), writes=[u_.t_Vbb])
        t_out = Tok()
        xts = [ar.alloc([D], F32) for _ in range(4)]
        t_xts = [Tok() for _ in range(4)]

        def issue(j):
            if j < len(tiles):
                tj = tiles[j]
                b.dma("sp", xts[j % 4], self.xsrc[tj * 128:(tj + 1) * 128, :], writes=[t_xts[j % 4]])
        issue(0)
        issue(1)

        def tile(idx, ti):
            u = sets[idx % 2]
            u.xt, u.t_xt = xts[idx % 4], t_xts[idx % 4]
            issue(idx + 2)
            pb = 4 * (idx % 2)
            PB_HT, PB_KV1, PB_KV2, PB_CT, PB_KVA, PB_KAT, PB_KBT = pb, pb + 1, pb + 2, pb, pb + 2, pb, pb + 1
            s = 1 if ti in self.ctx_tiles else 0
            yield from self.norm_mod_tile(u.xt, u.t_xt, gm[s], mod[(s, 0)], u.h.rearrange("p a b -> p (a b)"), u.t_h,
                                          u.scr, u.t_scr, u.st[:, 0:1], u.t_st)
            yield from self.transpose_rows(u.h, u.t_h, 8, 128, PB_HT, u.hT, u.t_hT)
            for kk in range(8):
                b.op("pe", lambda e, kk=kk: e.matmul(self.bank(PB_KV1)[:, 0:160], lhsT=u.hT[:, kk, :], rhs=wkv1[:, kk, :],
                                                     start=(kk == 0), stop=(kk == 7)),
                     reads=[u.t_hT, t_w], writes=[self.ptok[PB_KV1]])
            for kk in range(8):
                b.op("pe", lambda e, kk=kk: e.matmul(self.bank(PB_KV2)[:, 0:256], lhsT=u.hT[:, kk, :], rhs=wkv2[:, kk, :],
                                                     start=(kk == 0), stop=(kk == 7)),
                     reads=[u.t_hT, t_w], writes=[self.ptok[PB_KV2]])
            yield
            ckv = self.bank(PB_KV1)[:, 0:128]
            kr = self.bank(PB_KV1)[:, 128:160]
            b.op("act", lambda e: e.activation(out=u.junk, in_=ckv, func=AF.Square, accum_out=u.st[:, 1:2]),
                 reads=[self.ptok[PB_KV1]], writes=[u.t_junk, u.t_st])
            yield
            yield from self.rstd_from_ss(u.st[:, 1:2], 1, 128, u.t_st)
            b.op("dve", lambda e: e.scalar_tensor_tensor(out=u.ckvn[:, 0, :], in0=ckv, scalar=u.st[:, 1:2], in1=gckv,
                                                         op0=ALU.mult, op1=ALU.mult),
                 reads=[self.ptok[PB_KV1], u.t_st, t_g], writes=[u.t_ckvn])
            yield
            kbp = self.bank(PB_KV2)[:, 0:128].rearrange("p (h d) -> p h d", h=2, d=64)
            vbp = self.bank(PB_KV2)[:, 128:256].rearrange("p (h d) -> p h d", h=2, d=64)
            b.op("act", lambda e: e.activation(out=u.Vbb[:, :, 0:64], in_=vbp, func=AF.Copy), reads=[self.ptok[PB_KV2]],
                 writes=[u.t_Vbb])
            b.op("act", lambda e: e.activation(out=u.kbf, in_=kbp, func=AF.Copy), reads=[self.ptok[PB_KV2]],
                 writes=[u.t_kbf])
            yield
            b.dma("sp", self.Vb[:, ti, :], u.Vbb.rearrange("p a b -> p (a b)"), reads=[u.t_Vbb], writes=[t_out])
            b.op("act", lambda e: e.activation(out=u.junk[:, 0:32], in_=kr, func=AF.Square, accum_out=u.sskr),
                 reads=[self.ptok[PB_KV1]], writes=[u.t_junk, u.t_sskr])
            b.op("dve", lambda e: e.tensor_tensor(out=u.krf[:, 0, :], in0=kr, in1=gka[:, 64:96], op=ALU.mult),
                 reads=[self.ptok[PB_KV1], t_g2], writes=[u.t_krf])
            yield
            yield from self.transpose_rows(u.ckvn, u.t_ckvn, 1, 128, PB_CT, u.ckvnT, u.t_ckvnT, evac="dve")
            for j in range(2):
                b.op("pe", lambda e, j=j: e.matmul(self.bank(PB_KVA + j), lhsT=u.ckvnT[:, 0, :],
                                                   rhs=wukv[:, j * 512:(j + 1) * 512], start=True, stop=True),
                     reads=[u.t_ckvnT, t_w], writes=[self.ptok[PB_KVA + j]])
            yield
            kva = self.bank(PB_KVA, 2).rearrange("p (h d) -> p h d", h=8, d=128)
            t_kva = [self.ptok[PB_KVA], self.ptok[PB_KVA + 1]]
            knope = kva[:, :, 0:64]
            yield from self.rope_apply(u.krf, u.t_krf, 1, "a", ti, u.krr, u.t_krr, u.r1[:, 0:1, 0:32], u.r2[:, 0:1, 0:32],
                                       u.t_r1)
            b.op("act", lambda e: e.activation(out=u.kaf, in_=knope, func=AF.Copy), reads=t_kva, writes=[u.t_kaf])
            b.op("act", lambda e: e.activation(out=u.Vab[:, :, 0:64], in_=kva[:, :, 64:128], func=AF.Copy), reads=t_kva,
                 writes=[u.t_Vab])
            yield
            b.dma("sp", self.Va[:, ti, :], u.Vab.rearrange("p a b -> p (a b)"), reads=[u.t_Vab], writes=[t_out])
            b.op("act", lambda e: e.activation(out=u.sq, in_=u.kaf, func=AF.Square), reads=[u.t_kaf], writes=[u.t_sq])
            yield
            b.op("dve", lambda e: e.tensor_reduce(out=u.ss8, in_=u.sq, axis=AX.X, op=ALU.add), reads=[u.t_sq],
                 writes=[u.t_ss8])
            yield
            b.op("dve", lambda e: e.tensor_tensor(out=u.ss8, in0=u.ss8, in1=bc(u.sskr, [128, 8]), op=ALU.add),
                 reads=[u.t_ss8, u.t_sskr], writes=[u.t_ss8])
            yield
            yield from self.rstd_from_ss(u.ss8, 8, 96, u.t_ss8)
            b.op("dve", lambda e: e.tensor_tensor(out=u.kaf, in0=u.kaf, in1=bc(u.ss8.unsqueeze(2), [128, 8, 64]),
                                                  op=ALU.mult), reads=[u.t_kaf, u.t_ss8], writes=[u.t_kaf])
            yield
            b.op("dve", lambda e: e.tensor_tensor(out=u.Kab[:, :, 0:64], in0=u.kaf,
                                                  in1=bc(gka[:, 0:64].unsqueeze(1), [128, 8, 64]), op=ALU.mult),
                 reads=[u.t_kaf, t_g2], writes=[u.t_Kab])
            b.op("dve", lambda e: e.tensor_tensor(out=u.Kab[:, :, 64:96], in0=bc(u.krr, [128, 8, 32]),
                                                  in1=bc(u.ss8.unsqueeze(2), [128, 8, 32]), op=ALU.mult),
                 reads=[u.t_krr, u.t_ss8], writes=[u.t_Kab])
            yield
            yield from self.transpose_rows(u.Kab, u.t_Kab, 8, 96, PB_KAT, u.KaTs[0:96], u.t_KaTs)
            b.dma("sp", self.KaT[:, :, ti * 128:(ti + 1) * 128].rearrange("h d t -> d h t"), u.KaTs[0:96],
                  reads=[u.t_KaTs], writes=[t_out])
            yield from self.head_norm(u.kbf, u.t_kbf, 2, 64, None, gkb, t_g3, u.sq[:, 0:2, :], u.t_sq, u.ss2, u.t_ss2,
                                      u.kbf, u.t_kbf)
            yield from self.rope_apply(u.kbf, u.t_kbf, 2, "b", ti, u.Kbb, u.t_Kbb, u.r1[:, 0:2, :], u.r2[:, 0:2, :], u.t_r1)
            yield from self.transpose_rows(u.Kbb, u.t_Kbb, 2, 64, PB_KBT, u.KbTs[0:64], u.t_KbTs, evac="dve")
            b.dma("sp", self.KbT[:, :, ti * 128:(ti + 1) * 128].rearrange("h d t -> d h t"), u.KbTs[0:64],
                  reads=[u.t_KbTs], writes=[t_out])
            yield
        self.run_il((tile(i, t) for i, t in enumerate(tiles)), depth=2)
        b.barrier()
        ar.release(m0)

    def phase_q(self, tiles, setup):
        from types import SimpleNamespace as NS
        b, ar, l = self.b, self.ar, self.l
        m0 = ar.mark()
        mod, gm = setup
        wq1 = ar.alloc([8, 256], BF16)
        wq2 = ar.alloc([8, 512], BF16)
        wg = ar.alloc([8, 2048], BF16)
        wuq = ar.alloc([2, 768], BF16)
        t_w = Tok()
        wv = self.w_in[l].rearrange("(k p) c -> p k c", p=128)
        b.dma("pool", wq1, wv[:, :, C_CQ:C_CKV], writes=[t_w])
        b.dma("pool", wq2, wv[:, :, C_QB:C_KB], writes=[t_w])
        for j in range(4):
            b.dma("pool", wg[:, :, j * 512:(j + 1) * 512], wv[:, :, C_GA + j * 512:C_GA + (j + 1) * 512], writes=[t_w])
        b.dma("pool", wuq, self.w_uq[l].rearrange("(k p) c -> p k c", p=128), writes=[t_w])
        gcq = ar.alloc([256], F32)
        gqa = ar.alloc([96], F32)
        gqb = ar.alloc([64], F32)
        t_g = self.load_bcast(gcq, self.g_cq[l:l + 1, :])
        t_g2 = self.load_bcast(gqa, self.g_qa[l:l + 1, :])
        t_g3 = self.load_bcast(gqb, self.g_qb[l:l + 1, :])

        def mk():
            u = NS()
            for name, shp, dt_ in (("scr", [D], F32), ("st", [8], F32), ("h", [8, 128], BF16),
                                   ("hT", [8, 128], BF16), ("cqn", [2, 128], BF16), ("cqnT", [2, 128], BF16),
                                   ("ss8", [8], F32), ("ss8b", [8], F32), ("sq", [8, 96], F32), ("qaf", [8, 96], F32),
                                   ("qbf", [8, 64], F32), ("r1", [8, 64], F32), ("r2", [8, 64], F32),
                                   ("Qab", [8, 96], BF16), ("Qbb", [8, 64], BF16), ("QaTs", [8, 128], BF16),
                                   ("QbTs", [8, 128], BF16), ("Gsb", [2048], BF16), ("junk", [256], F32)):
                setattr(u, name, ar.alloc(shp, dt_))
                setattr(u, "t_" + name, Tok())
            return u
        sets = [mk(), mk()]
        t_out = Tok()
        xts = [ar.alloc([D], F32) for _ in range(4)]
        t_xts = [Tok() for _ in range(4)]

        def issue(j):
            if j < len(tiles):
                tj = tiles[j]
                b.dma("sp", xts[j % 4], self.xsrc[tj * 128:(tj + 1) * 128, :], writes=[t_xts[j % 4]])
        issue(0)
        issue(1)

        def tile(idx, ti):
            u = sets[idx % 2]
            u.xt, u.t_xt = xts[idx % 4], t_xts[idx % 4]
            issue(idx + 2)
            pb = 4 * (idx % 2)
            PB_HT, PB_CQ, PB_QB, PB_CT, PB_QA, PB_QTA, PB_QTB = pb, pb + 1, pb + 2, pb, pb + 2, pb + 2, pb + 3
            s = 1 if ti in self.ctx_tiles else 0
            yield from self.norm_mod_tile(u.xt, u.t_xt, gm[s], mod[(s, 0)], u.h.rearrange("p a b -> p (a b)"), u.t_h,
                                          u.scr, u.t_scr, u.st[:, 0:1], u.t_st)
            yield from self.transpose_rows(u.h, u.t_h, 8, 128, PB_HT, u.hT, u.t_hT)
            for kk in range(8):
                b.op("pe", lambda e, kk=kk: e.matmul(self.bank(PB_CQ)[:, 0:256], lhsT=u.hT[:, kk, :], rhs=wq1[:, kk, :],
                                                     start=(kk == 0), stop=(kk == 7)),
                     reads=[u.t_hT, t_w], writes=[self.ptok[PB_CQ]])
            for kk in range(8):
                b.op("pe", lambda e, kk=kk: e.matmul(self.bank(PB_QB), lhsT=u.hT[:, kk, :], rhs=wq2[:, kk, :],
                                                     start=(kk == 0), stop=(kk == 7)),
                     reads=[u.t_hT, t_w], writes=[self.ptok[PB_QB]])
            yield
            cq = self.bank(PB_CQ)[:, 0:256]
            b.op("act", lambda e: e.activation(out=u.junk, in_=cq, func=AF.Square, accum_out=u.st[:, 1:2]),
                 reads=[self.ptok[PB_CQ]], writes=[u.t_junk, u.t_st])
            qbp = self.bank(PB_QB).rearrange("p (h d) -> p h d", h=8, d=64)
            b.op("act", lambda e: e.activation(out=u.qbf, in_=qbp, func=AF.Copy), reads=[self.ptok[PB_QB]],
                 writes=[u.t_qbf])
            yield
            yield from self.rstd_from_ss(u.st[:, 1:2], 1, 256, u.t_st)
            b.op("dve", lambda e: e.scalar_tensor_tensor(out=u.cqn.rearrange("p a b -> p (a b)"), in0=cq,
                                                         scalar=u.st[:, 1:2], in1=gcq, op0=ALU.mult, op1=ALU.mult),
                 reads=[self.ptok[PB_CQ], u.t_st, t_g], writes=[u.t_cqn])
            yield
            yield from self.transpose_rows(u.cqn, u.t_cqn, 2, 128, PB_CT, u.cqnT, u.t_cqnT, evac="dve")
            for j, (c0, c1) in enumerate(((0, 512), (512, 768))):
                for kk in range(2):
                    b.op("pe", lambda e, j=j, kk=kk, c0=c0, c1=c1: e.matmul(
                        self.bank(PB_QA + j)[:, 0:c1 - c0], lhsT=u.cqnT[:, kk, :], rhs=wuq[:, kk, c0:c1],
                        start=(kk == 0), stop=(kk == 1)), reads=[u.t_cqnT, t_w], writes=[self.ptok[PB_QA + j]])
            for j in range(4):
                pbg = PB_CQ if j % 2 == 0 else PB_CT
                for kk in range(8):
                    b.op("pe", lambda e, kk=kk, j=j, pb=pbg: e.matmul(self.bank(pb), lhsT=u.hT[:, kk, :],
                                                                     rhs=wg[:, kk, j * 512:(j + 1) * 512],
                                                                     start=(kk == 0), stop=(kk == 7)),
                         reads=[u.t_hT, t_w], writes=[self.ptok[pbg]])
                b.op("act", lambda e, j=j, pb=pbg: e.activation(out=u.Gsb[:, j * 512:(j + 1) * 512], in_=self.bank(pb),
                                                                func=AF.Sigmoid), reads=[self.ptok[pbg]], writes=[u.t_Gsb])
                if j == 0:
                    qa = self.bank(PB_QA, 2)[:, 0:768].rearrange("p (h d) -> p h d", h=8, d=96)
                    b.op("act", lambda e: e.activation(out=u.qaf, in_=qa, func=AF.Copy),
                         reads=[self.ptok[PB_QA], self.ptok[PB_QA + 1]], writes=[u.t_qaf])
                yield
            b.dma("sp", self.Gs[ti * 128:(ti + 1) * 128, :], u.Gsb, reads=[u.t_Gsb], writes=[t_out])
            yield from self.head_norm(u.qaf, u.t_qaf, 8, 96, None, gqa, t_g2, u.sq, u.t_sq, u.ss8, u.t_ss8, u.qaf, u.t_qaf)
            b.op("act", lambda e: e.activation(out=u.Qab[:, :, 0:64], in_=u.qaf[:, :, 0:64], func=AF.Copy),
                 reads=[u.t_qaf], writes=[u.t_Qab])
            yield from self.rope_apply(u.qaf[:, :, 64:96], u.t_qaf, 8, "a", ti, u.Qab[:, :, 64:96], u.t_Qab,
                                       u.r1[:, :, 0:32], u.r2[:, :, 0:32], u.t_r1)
            yield from self.transpose_rows(u.Qab, u.t_Qab, 8, 96, PB_QTA, u.QaTs[0:96], u.t_QaTs)
            b.dma("sp", self.QaT[:, :, ti * 128:(ti + 1) * 128].rearrange("h d t -> d h t"), u.QaTs[0:96],
                  reads=[u.t_QaTs], writes=[t_out])
            yield from self.head_norm(u.qbf, u.t_qbf, 8, 64, None, gqb, t_g3, u.sq[:, :, 0:64], u.t_sq, u.ss8b, u.t_ss8b,
                                      u.qbf, u.t_qbf)
            yield from self.rope_apply(u.qbf, u.t_qbf, 8, "b", ti, u.Qbb, u.t_Qbb, u.r1, u.r2, u.t_r1)
            yield from self.transpose_rows(u.Qbb, u.t_Qbb, 8, 64, PB_QTB, u.QbTs[0:64], u.t_QbTs, evac="dve")
            b.dma("sp", self.QbT[:, :, ti * 128:(ti + 1) * 128].rearrange("h d t -> d h t"), u.QbTs[0:64],
                  reads=[u.t_QbTs], writes=[t_out])
            yield
        self.run_il((tile(i, t) for i, t in enumerate(tiles)), depth=2)
        b.barrier()
        ar.release(m0)

    def phase_attn(self, blocks):
        b, ar, l = self.b, self.ar, self.l
        m0 = ar.mark()
        Vas = ar.alloc([NKT, 8, 66], BF16)
        Vbs = ar.alloc([NKT, 2, 66], BF16)
        t_V = Tok()
        for k0 in range(0, NKT, 9):
            k1 = min(NKT, k0 + 9)
            b.dma("sp", Vas[:, k0:k1].rearrange("p k h d -> p k (h d)"), self.Va[:, k0:k1, :], writes=[t_V])
        b.dma("sp", Vbs.rearrange("p k h d -> p k (h d)"), self.Vb, writes=[t_V])
        KT = [ar.alloc([NKEY], BF16) for _ in range(2)]
        t_KT = [Tok(), Tok()]
        QT = [ar.alloc([self.NTOK], BF16) for _ in range(2)]
        t_QT = [Tok(), Tok()]
        for i_ in range(2):
            b.op("dve", lambda e, i_=i_: e.memset(KT[i_][64:128], 0.0), writes=[t_KT[i_]])
            b.op("dve", lambda e, i_=i_: e.memset(QT[i_][64:128], 0.0), writes=[t_QT[i_]])
        PT = [ar.alloc([1024], BF16) for _ in range(3)]
        t_PT = [Tok() for _ in range(3)]
        OTs = [ar.alloc([512], BF16) for _ in range(2)]
        t_OTs = [Tok(), Tok()]
        rd = [ar.alloc([512], F32) for _ in range(2)]
        t_rd = [Tok(), Tok()]
        otf = [ar.alloc([512], F32) for _ in range(2)]
        t_otf = [Tok(), Tok()]
        onesb = ar.alloc([64], BF16)
        t_onesb = Tok()
        b.op("dve", lambda e: e.memset(onesb, 1.0), writes=[t_onesb])
        rdh = [ar.alloc([512], BF16) for _ in range(2)]
        rdl = [ar.alloc([512], BF16) for _ in range(2)]
        PS_S, PS_O, PS_BC = (0, 1, 2, 3), (4, 5), 6
        t_out = Tok()
        from types import SimpleNamespace as NS

        def _ldh(hj):
            if hj < 8:
                dj, ks, qs = 96, self.KaT[hj], self.QaT[hj]
            else:
                dj, ks, qs = 64, self.KbT[(hj - 8) // 4], self.QbT[hj - 8]
            if hj in (8, 9):
                b.op("dve", lambda e: e.memset(KT[hj % 2][64:128], 0.0), writes=[t_KT[hj % 2]])
                b.op("dve", lambda e: e.memset(QT[hj % 2][64:128], 0.0), writes=[t_QT[hj % 2]])
            b.dma("sp", KT[hj % 2][0:dj], ks, writes=[t_KT[hj % 2]])
            b.dma("sp", QT[hj % 2][0:dj], qs, writes=[t_QT[hj % 2]])

        items = []
        octr = 0
        for hh in range(16):
            if hh < 8:
                scale = SCALE_A
                vsl = lambda kt, hh=hh: Vas[:, kt, hh, 0:65]
            else:
                scale = SCALE_B
                vsl = lambda kt, g=hh - 8: Vbs[:, kt, g // 4, 0:65]
            for bi, (q0, nq, kts) in enumerate(blocks):
                groups = [kts[i:i + 2] for i in range(0, len(kts), 2)]
                for gi, grp in enumerate(groups):
                    items.append(NS(hh=hh, hb=hh % 2, q0=q0, nq=nq, ob=PS_O[octr % 2], op_=octr % 2, gi=gi, grp=grp,
                                    nk=len(kts), first_of_head=(bi == 0 and gi == 0), last=(gi == len(groups) - 1),
                                    vsl=vsl, scale=scale, idx=len(items)))
                octr += 1
        n = len(items)
        deferred = []

        def emit_S(it):
            sp_ = it.idx % 2
            pt = it.idx % 3
            it.pt = pt
            sbanks = [PS_S[2 * sp_ + j] for j in range(len(it.grp))]
            for j, kt in enumerate(it.grp):
                b.op("pe", lambda e, sb=sbanks[j], kt=kt: e.matmul(
                    self.bank(sb)[:, 0:it.nq], lhsT=KT[it.hb][:, kt * 128:(kt + 1) * 128],
                    rhs=QT[it.hb][:, it.q0:it.q0 + it.nq], start=True, stop=True),
                    reads=[t_KT[it.hb], t_QT[it.hb]], writes=[self.ptok[sbanks[j]]])
            if it.nq == 512 and len(it.grp) == 2:
                b.op("act", lambda e: e.activation(out=PT[pt], in_=self.bank(sbanks[0], 2), func=AF.Exp, scale=it.scale),
                     reads=[self.ptok[x] for x in sbanks], writes=[t_PT[pt]])
            else:
                for j in range(len(it.grp)):
                    b.op("act", lambda e, j=j: e.activation(
                        out=PT[pt][:, j * 512:j * 512 + it.nq], in_=self.bank(sbanks[j])[:, 0:it.nq], func=AF.Exp,
                        scale=it.scale), reads=[self.ptok[sbanks[j]]], writes=[t_PT[pt]])

        def emit_PV(it):
            for j, pkt in enumerate(it.grp):
                pi = 2 * it.gi + j
                b.op("pe", lambda e, j=j, pkt=pkt, pi=pi: e.matmul(
                    self.bank(it.ob)[0:65, 0:it.nq], lhsT=it.vsl(pkt), rhs=PT[it.pt][:, j * 512:j * 512 + it.nq],
                    start=(pi == 0), stop=(pi == it.nk - 1)), reads=[t_V, t_PT[it.pt]], writes=[self.ptok[it.ob]])

        def emit_norm_head(it):
            ob, nq, op_ = it.ob, it.nq, it.op_
            b.op("dve", lambda e: e.reciprocal(out=rd[op_][64:65, 0:nq], in_=self.bank(ob)[64:65, 0:nq]),
                 reads=[self.ptok[ob]], writes=[t_rd[op_]])
            b.op("dve", lambda e: e.tensor_copy(out=otf[op_][0:64, 0:nq], in_=self.bank(ob)[0:64, 0:nq]),
                 reads=[self.ptok[ob]], writes=[t_otf[op_]])
            b.op("dve", lambda e: e.tensor_copy(out=rdh[op_][64:65, 0:nq], in_=rd[op_][64:65, 0:nq]),
                 reads=[t_rd[op_]], writes=[t_rd[op_]])
            b.op("dve", lambda e: e.tensor_tensor(out=rdl[op_][64:65, 0:nq], in0=rd[op_][64:65, 0:nq],
                                                  in1=rdh[op_][64:65, 0:nq], op=ALU.subtract),
                 reads=[t_rd[op_]], writes=[t_rd[op_]])

            def tail():
                b.op("pe", lambda e: e.matmul(self.bank(PS_BC)[0:64, 0:nq], lhsT=onesb[64:65, 0:64],
                                              rhs=rdh[op_][64:65, 0:nq], start=True, stop=False),
                     reads=[t_rd[op_], t_onesb], writes=[self.ptok[PS_BC]])
                b.op("pe", lambda e: e.matmul(self.bank(PS_BC)[0:64, 0:nq], lhsT=onesb[64:65, 0:64],
                                              rhs=rdl[op_][64:65, 0:nq], start=False, stop=True),
                     reads=[t_rd[op_], t_onesb], writes=[self.ptok[PS_BC]])
                b.op("dve", lambda e: e.tensor_tensor(
                    out=OTs[op_][0:64, 0:nq], in0=otf[op_][0:64, 0:nq], in1=self.bank(PS_BC)[0:64, 0:nq],
                    op=ALU.mult), reads=[t_otf[op_], self.ptok[PS_BC]], writes=[t_OTs[op_]])
                b.dma("sp", self.OT[it.hh, :, it.q0:it.q0 + nq], OTs[op_][0:64, 0:nq], reads=[t_OTs[op_]],
                      writes=[t_out])
            return tail

        for t in range(n + 1):
            if t < n:
                it = items[t]
                if it.first_of_head:
                    if it.hh == 0:
                        _ldh(0)
                    if it.hh + 1 < 16:
                        _ldh(it.hh + 1)
                emit_S(it)
            for due, fn in [x for x in deferred if x[0] <= t]:
                fn()
            deferred = [x for x in deferred if x[0] > t]
            if t >= 1:
                pit = items[t - 1]
                emit_PV(pit)
                if pit.last:
                    deferred.append((t + 6, emit_norm_head(pit)))
        for due, fn in deferred:
            fn()
        b.barrier()
        ar.release(m0)

    def phase_merge(self, tiles):
        from types import SimpleNamespace as NS
        b, ar, l = self.b, self.ar, self.l
        m0 = ar.mark()
        mod = self.emit_mod([2])
        woa = ar.alloc([4, 1024], BF16)
        wob = ar.alloc([4, 1024], BF16)
        wout = ar.alloc([8, 1024], BF16)
        t_w = Tok()
        b.dma("pool", woa, self.w_oa[l].rearrange("(k p) c -> p k c", p=128), writes=[t_w])
        b.dma("pool", wob, self.w_ob[l].rearrange("(k p) c -> p k c", p=128), writes=[t_w])
        b.dma("pool", wout, self.w_out[l].rearrange("(k p) c -> p k c", p=128), writes=[t_w])

        def mk():
            u = NS()
            for name, shp, dt_ in (("t1", [D], F32), ("t2", [D], F32), ("y", [8, 128], BF16), ("yT", [8, 128], BF16)):
                setattr(u, name, ar.alloc(shp, dt_))
                setattr(u, "t_" + name, Tok())
            return u
        sets = [mk(), mk()]
        OT2 = self.OT.rearrange("h d t -> (h d) t")
        lds = []
        for _ in range(4):
            lds.append(NS(OTt=ar.alloc([8, 128], BF16), Gt=ar.alloc([2048], BF16), xt=ar.alloc([D], F32),
                          t_OTt=Tok(), t_Gt=Tok(), t_xt=Tok()))

        def issue(j):
            if j < len(tiles):
                tj, v = tiles[j], lds[j % 4]
                b.dma("sp", v.OTt, OT2[:, tj * 128:(tj + 1) * 128].rearrange("(k p) t -> p k t", p=128), writes=[v.t_OTt])
                b.dma("sp", v.Gt, self.Gs[tj * 128:(tj + 1) * 128, :], writes=[v.t_Gt])
                b.dma("sp", v.xt, self.xsrc[tj * 128:(tj + 1) * 128, :], writes=[v.t_xt])
        issue(0)
        issue(1)

        def tile(idx, ti):
            u = sets[idx % 2]
            v = lds[idx % 4]
            u.OTt, u.Gt, u.xt, u.t_OTt, u.t_Gt, u.t_xt = v.OTt, v.Gt, v.xt, v.t_OTt, v.t_Gt, v.t_xt
            s = 1 if ti in self.ctx_tiles else 0
            issue(idx + 2)
            pbase = 4 * (idx % 2)
            for br, w_ in ((0, woa), (1, wob)):
                for j in range(2):
                    pb = pbase + 2 * br + j
                    for kk in range(4):
                        b.op("pe", lambda e, pb=pb, kk=kk, br=br, w_=w_, j=j: e.matmul(
                            self.bank(pb), lhsT=u.OTt[:, 4 * br + kk, :], rhs=w_[:, kk, j * 512:(j + 1) * 512],
                            start=(kk == 0), stop=(kk == 3)), reads=[u.t_OTt, t_w], writes=[self.ptok[pb]])
            yield
            b.op("dve", lambda e: e.tensor_tensor(out=u.t1, in0=self.bank(pbase, 2), in1=u.Gt[:, 0:1024], op=ALU.mult),
                 reads=[self.ptok[pbase], self.ptok[pbase + 1], u.t_Gt], writes=[u.t_t1])
            yield
            b.op("dve", lambda e: e.tensor_tensor(out=u.t2, in0=self.bank(pbase + 2, 2), in1=u.Gt[:, 1024:2048],
                                                  op=ALU.mult),
                 reads=[self.ptok[pbase + 2], self.ptok[pbase + 3], u.t_Gt], writes=[u.t_t2])
            yield
            b.op("dve", lambda e: e.tensor_tensor(out=u.y.rearrange("p a b -> p (a b)"), in0=u.t1, in1=u.t2, op=ALU.add),
                 reads=[u.t_t1, u.t_t2], writes=[u.t_y])
            yield
            yield from self.transpose_rows(u.y, u.t_y, 8, 128, pbase, u.yT, u.t_yT)
            for j in range(2):
                for kk in range(8):
                    b.op("pe", lambda e, j=j, kk=kk: e.matmul(self.bank(pbase + 1 + j), lhsT=u.yT[:, kk, :],
                                                              rhs=wout[:, kk, j * 512:(j + 1) * 512],
                                                              start=(kk == 0), stop=(kk == 7)),
                         reads=[u.t_yT, t_w], writes=[self.ptok[pbase + 1 + j]])
            yield
            gt1, t_gt1 = mod[(s, 2)]
            b.op("dve", lambda e: e.tensor_tensor(out=u.t1, in0=self.bank(pbase + 1, 2), in1=gt1, op=ALU.mult),
                 reads=[self.ptok[pbase + 1], self.ptok[pbase + 2], t_gt1], writes=[u.t_t1])
            yield
            b.op("dve", lambda e: e.tensor_tensor(out=self.acc[:, idx, :], in0=u.t1, in1=u.xt, op=ALU.add),
                 reads=[u.t_t1, u.t_xt], writes=[self.t_acc[idx]])
            yield
        self.run_il((tile(i, t) for i, t in enumerate(tiles)), depth=2)
        b.barrier()
        ar.release(m0)

    def phase_moe(self, tiles, dst):
        from types import SimpleNamespace as NS
        b, ar, l = self.b, self.ar, self.l
        NT = 17
        ntile = len(tiles)
        isctx = [t in self.ctx_tiles for t in tiles]
        has_ctx = any(isctx)
        m0 = ar.mark()
        fT = ar.alloc([8, NT * 128], BF16)
        t_fT = [Tok() for _ in range(NT)]
        gate = ar.alloc([NT, 16], F32)
        t_gate = [Tok() for _ in range(NT)]
        modg = self.emit_mod([5])
        m1 = ar.mark()
        mod = self.emit_mod([3, 4])
        gff = ar.alloc([D], F32)
        t_gff = self.load_bcast(gff, self.g_ffn[l:l + 1, :])
        gm = {}
        for s in (0, 1):
            ap, tk = mod[(s, 4)]
            b.op("dve", lambda e, ap=ap: e.scalar_tensor_tensor(out=ap, in0=ap, scalar=1.0, in1=gff, op0=ALU.add,
                                                                op1=ALU.mult), reads=[tk, t_gff], writes=[tk])
            gm[s] = (ap, tk)
        wr = ar.alloc([8, 16], F32)
        t_wr = Tok()
        b.dma("sp", wr, self.w_router.rearrange("(k p) e -> p k e", p=128), writes=[t_wr])
        brt = ar.alloc([16], F32)
        t_brt = self.load_bcast(brt, self.b_router)

        def mk():
            u = NS()
            for name, shp, dt_ in (("ff", [8, 128], F32), ("scr", [D], F32), ("st", [8], F32), ("fT32", [8, 128], F32),
                                   ("sc", [4, 4], F32), ("sel", [4, 4], F32), ("eq", [4, 4], F32), ("g2", [4, 4], F32),
                                   ("m1t", [4], F32), ("m2t", [4], F32), ("gs", [4], F32), ("gmx", [2], F32),
                                   ("gmask", [4], F32)):
                setattr(u, name, ar.alloc(shp, dt_))
                setattr(u, "t_" + name, Tok())
            return u
        sets = [mk(), mk()]

        def ptile(ti):
            u = sets[ti % 2]
            s = 1 if isctx[ti] else 0
            pb0 = 4 * (ti % 2)
            yield from self.norm_mod_tile(self.acc[:, ti, :], self.t_acc[ti], gm[s], mod[(s, 3)],
                                          u.ff.rearrange("p a b -> p (a b)"), u.t_ff, u.scr, u.t_scr, u.st[:, 0:1], u.t_st)
            for kk in range(8):
                b.op("pe", lambda e, kk=kk: e.transpose(self.bank(pb0, 2)[:, kk * 128:(kk + 1) * 128], u.ff[:, kk, :],
                                                        self.identf), reads=[u.t_ff, self.t_identf],
                     writes=[self.ptok[pb0], self.ptok[pb0 + 1]])
            yield
            src = self.bank(pb0, 2).rearrange("p (a b) -> p a b", a=8, b=128)
            b.op("act", lambda e: e.activation(out=u.fT32, in_=src, func=AF.Copy),
                 reads=[self.ptok[pb0], self.ptok[pb0 + 1]], writes=[u.t_fT32])
            b.op("dve", lambda e: e.tensor_copy(out=fT[:, :, ti * 128:(ti + 1) * 128], in_=src),
                 reads=[self.ptok[pb0], self.ptok[pb0 + 1]], writes=[t_fT[ti]])
            yield
            for kk in range(8):
                b.op("pe", lambda e, kk=kk: e.matmul(self.bank(pb0 + 2)[:, 0:16], lhsT=u.fT32[:, kk, :], rhs=wr[:, kk, :],
                                                     start=(kk == 0), stop=(kk == 7)), reads=[u.t_fT32, t_wr],
                     writes=[self.ptok[pb0 + 2]])
            yield
            scf = u.sc.rearrange("p a b -> p (a b)")
            g2f = u.g2.rearrange("p a b -> p (a b)")
            steps = [
                ("act", lambda e: e.activation(out=scf, in_=self.bank(pb0 + 2)[:, 0:16], func=AF.Sigmoid),
                 [self.ptok[pb0 + 2]], [u.t_sc]),
                ("dve", lambda e: e.tensor_tensor(out=u.sel.rearrange("p a b -> p (a b)"), in0=scf, in1=brt, op=ALU.add),
                 [u.t_sc, t_brt], [u.t_sel]),
                ("dve", lambda e: e.tensor_reduce(out=u.m1t, in_=u.sel, axis=AX.X, op=ALU.max), [u.t_sel], [u.t_m1t]),
                ("dve", lambda e: e.tensor_tensor(out=u.eq, in0=u.sel, in1=bc(u.m1t.unsqueeze(2), [128, 4, 4]),
                                                  op=ALU.is_equal), [u.t_sel, u.t_m1t], [u.t_eq]),
                ("dve", lambda e: e.scalar_tensor_tensor(out=u.g2, in0=u.eq, scalar=-1e30, in1=u.sel, op0=ALU.mult,
                                                         op1=ALU.add), [u.t_eq, u.t_sel], [u.t_g2]),
                ("dve", lambda e: e.tensor_reduce(out=u.m2t, in_=u.g2, axis=AX.X, op=ALU.max), [u.t_g2], [u.t_m2t]),
                ("dve", lambda e: e.tensor_tensor(out=u.gs, in0=u.m1t, in1=u.m2t, op=ALU.add), [u.t_m1t, u.t_m2t],
                 [u.t_gs]),
                ("dve", lambda e: e.tensor_reduce(out=u.gmx[:, 0:1], in_=u.gs, axis=AX.X, op=ALU.max), [u.t_gs],
                 [u.t_gmx]),
                ("dve", lambda e: e.tensor_scalar(out=u.gmask, in0=u.gs, scalar1=u.gmx[:, 0:1], scalar2=None,
                                                  op0=ALU.is_equal), [u.t_gs, u.t_gmx], [u.t_gmask]),
                ("dve", lambda e: e.tensor_tensor(out=u.eq, in0=u.sel, in1=bc(u.m2t.unsqueeze(2), [128, 4, 4]),
                                                  op=ALU.is_ge), [u.t_sel, u.t_m2t], [u.t_eq]),
                ("dve", lambda e: e.tensor_tensor(out=u.eq, in0=u.eq, in1=bc(u.gmask.unsqueeze(2), [128, 4, 4]),
                                                  op=ALU.mult), [u.t_eq, u.t_gmask], [u.t_eq]),
                ("dve", lambda e: e.tensor_tensor(out=u.g2, in0=u.eq, in1=u.sc, op=ALU.mult), [u.t_eq, u.t_sc], [u.t_g2]),
                ("dve", lambda e: e.tensor_reduce(out=u.gmx[:, 1:2], in_=g2f, axis=AX.X, op=ALU.add), [u.t_g2],
                 [u.t_gmx]),
                ("dve", lambda e: e.reciprocal(out=u.gmx[:, 1:2], in_=u.gmx[:, 1:2]), [u.t_gmx], [u.t_gmx]),
                ("dve", lambda e: e.tensor_scalar(out=gate[:, ti, :], in0=g2f, scalar1=u.gmx[:, 1:2], scalar2=None,
                                                  op0=ALU.mult), [u.t_g2, u.t_gmx], [t_gate[ti]]),
            ]
            for eng_, fn_, rd_, wr_ in steps:
                b.op(eng_, fn_, reads=rd_, writes=wr_)
                yield
        self.run_il((ptile(t) for t in range(ntile)), depth=2)
        b.barrier()
        ar.release(m1)
        wgs = [ar.alloc([8, 512], BF16) for _ in range(2)]
        wus = [ar.alloc([8, 512], BF16) for _ in range(2)]
        t_wgu = [Tok(), Tok()]
        wd32 = ar.alloc([4, 1024], F32)
        t_wd32 = Tok()
        wdl = [ar.alloc([4, 1024], BF16) for _ in range(2)]
        wdc = [ar.alloc([4, 1024], BF16) for _ in range(2)]
        t_wd = [Tok(), Tok()]
        sg = [ar.alloc([512], F32) for _ in range(2)]
        t_sg = [Tok(), Tok()]
        AT = [ar.alloc([4, 512], BF16) for _ in range(2)]
        t_AT = [Tok(), Tok()]
        blocks = [(q * 512, min(512, ntile * 128 - q * 512)) for q in range((ntile + 3) // 4)]
        gctr = 0
        yctr = 0
        actr = 0
        for e_ in range(17):
            wb = e_ % 2
            if e_ == 0:
                sg_, su_, sd_ = self.w_s_gate[l], self.w_s_up[l], self.w_s_down[l]
            else:
                sg_, su_, sd_ = self.w_e_gate[l, e_ - 1], self.w_e_up[l, e_ - 1], self.w_e_down[l, e_ - 1]
            def _ldw(ej, only_d=False, only_gu=False):
                if ej == 0:
                    a_, b_, c_ = self.w_s_gate[l], self.w_s_up[l], self.w_s_down[l]
                else:
                    a_, b_, c_ = self.w_e_gate[l, ej - 1], self.w_e_up[l, ej - 1], self.w_e_down[l, ej - 1]
                if not only_d:
                    b.dma("pool", wgs[ej % 2], a_.rearrange("(k p) f -> p k f", p=128), writes=[t_wgu[ej % 2]])
                    b.dma("pool", wus[ej % 2], b_.rearrange("(k p) f -> p k f", p=128), writes=[t_wgu[ej % 2]])
                if not only_gu:
                    b.dma("sp", wd32, c_.rearrange("(k p) c -> p k c", p=128), writes=[t_wd32])
            if e_ == 0:
                _ldw(0)
            if e_ + 1 < 17:
                _ldw(e_ + 1, only_gu=True)
            b.op("dve", lambda e, wb=wb: e.tensor_tensor(out=wdl[wb], in0=wd32,
                                                         in1=bc(modg[(0, 5)][0].unsqueeze(1), [128, 4, 1024]), op=ALU.mult),
                 reads=[t_wd32, modg[(0, 5)][1]], writes=[t_wd[wb]])
            if has_ctx:
                b.op("dve", lambda e, wb=wb: e.tensor_tensor(out=wdc[wb], in0=wd32,
                                                             in1=bc(modg[(1, 5)][0].unsqueeze(1), [128, 4, 1024]),
                                                             op=ALU.mult),
                     reads=[t_wd32, modg[(1, 5)][1]], writes=[t_wd[wb]])
            if e_ + 1 < 17:
                _ldw(e_ + 1, only_d=True)
            for (t0, nt) in blocks:
                ab = actr % 2
                actr += 1
                tiles = list(range(t0 // 128, (t0 + nt) // 128))
                for fc in range(4):
                    gb = gctr % 2
                    gctr += 1
                    for kk in range(8):
                        b.op("pe", lambda e, gb=gb, kk=kk, fc=fc, wb=wb, t0=t0, nt=nt: e.matmul(
                            self.bank(gb)[:, 0:nt], lhsT=wgs[wb][:, kk, fc * 128:(fc + 1) * 128], rhs=fT[:, kk, t0:t0 + nt],
                            start=(kk == 0), stop=(kk == 7)), reads=[t_wgu[wb]] + [t_fT[t] for t in tiles],
                            writes=[self.ptok[gb]])
                    for kk in range(8):
                        b.op("pe", lambda e, gb=gb, kk=kk, fc=fc, wb=wb, t0=t0, nt=nt: e.matmul(
                            self.bank(2 + gb)[:, 0:nt], lhsT=wus[wb][:, kk, fc * 128:(fc + 1) * 128],
                            rhs=fT[:, kk, t0:t0 + nt], start=(kk == 0), stop=(kk == 7)),
                            reads=[t_wgu[wb]] + [t_fT[t] for t in tiles], writes=[self.ptok[2 + gb]])
                    b.op("act", lambda e, gb=gb, nt=nt: e.activation(out=sg[gb][:, 0:nt], in_=self.bank(gb)[:, 0:nt],
                                                                      func=AF.Silu), reads=[self.ptok[gb]], writes=[t_sg[gb]])
                    b.op("dve", lambda e, gb=gb, nt=nt, ab=ab, fc=fc: e.tensor_tensor(
                        out=AT[ab][:, fc, 0:nt], in0=sg[gb][:, 0:nt], in1=self.bank(2 + gb)[:, 0:nt], op=ALU.mult),
                        reads=[t_sg[gb], self.ptok[2 + gb]], writes=[t_AT[ab]])
                for i, ti in enumerate(tiles):
                    yb = 4 + 2 * (yctr % 2)
                    yctr += 1
                    wd_ = wdc[wb] if isctx[ti] else wdl[wb]
                    for j in range(2):
                        for fc in range(4):
                            b.op("pe", lambda e, yb=yb, j=j, fc=fc, ab=ab, i=i, wd_=wd_: e.matmul(
                                self.bank(yb + j), lhsT=AT[ab][:, fc, i * 128:(i + 1) * 128],
                                rhs=wd_[:, fc, j * 512:(j + 1) * 512], start=(fc == 0), stop=(fc == 3)),
                                reads=[t_AT[ab], t_wd[wb]], writes=[self.ptok[yb + j]])
                    if e_ == 0:
                        b.op("dve", lambda e, yb=yb, ti=ti: e.tensor_tensor(out=self.acc[:, ti, :], in0=self.bank(yb, 2),
                                                                            in1=self.acc[:, ti, :], op=ALU.add),
                             reads=[self.ptok[yb], self.ptok[yb + 1]], writes=[self.t_acc[ti]])
                    else:
                        b.op("dve", lambda e, yb=yb, ti=ti, e_=e_: e.scalar_tensor_tensor(
                            out=self.acc[:, ti, :], in0=self.bank(yb, 2), scalar=gate[:, ti, e_ - 1:e_],
                            in1=self.acc[:, ti, :], op0=ALU.mult, op1=ALU.add),
                            reads=[self.ptok[yb], self.ptok[yb + 1], t_gate[ti]], writes=[self.t_acc[ti]])
        t_out = Tok()
        for ti in range(ntile):
            b.dma("sp", dst(ti), self.acc[:, ti, :], reads=[self.t_acc[ti]], writes=[t_out])
        b.barrier()
        ar.release(m0)

    def build(self):
        self.setup_consts()
        allk = list(range(NKT))
        if self.mode == "A":
            self.phase_kv(list(range(NT)))
        elif self.mode == "B":
            tiles = list(range(NLAT_T if self.last else NT))
            blocks = [(q * 512, 512, allk) for q in range(4)]
            if not self.last:
                blocks.append((2048, 128, self.ctx_keys))
            self.phase_q(tiles)
            self.phase_attn(blocks)
            self.acc = self.ar.alloc([NT, D], F32)
            self.t_acc = [Tok() for _ in range(NT)]
            self.phase_merge(tiles)
            self.phase_moe(tiles, lambda i: self.xout[i * 128:(i + 1) * 128, :])
        else:
            self.l, self.last, self.xsrc = 0, False, self.xin
            alltiles = list(range(34))
            msetup = self.ar.mark()
            setup = self._common_setup()
            self.phase_kvq(alltiles, alltiles, setup)
            self.ar.release(msetup)
            blocks = [(q * 512, 512, allk) for q in range(8)] + [(4096, 256, self.ctx_keys)]
            self.phase_attn(blocks)
            macc = self.ar.mark()
            self.acc = self.ar.alloc([NT, D], F32)
            self.t_acc = [Tok() for _ in range(NT)]
            for tl in (alltiles[0:17], alltiles[17:34]):
                self.phase_merge(tl)
                self.phase_moe(tl, lambda i, tl=tl: self.xs1[tl[i] * 128:(tl[i] + 1) * 128, :])
            self.b.barrier()
            self.ar.release(macc)
            self.l, self.last, self.xsrc = 1, True, self.xs1
            own = list(range(16))
            msetup = self.ar.mark()
            setup = self._common_setup()
            self.phase_kvq(alltiles, own, setup)
            self.ar.release(msetup)
            self.phase_attn([(q * 512, 512, allk) for q in range(4)])
            self.acc = self.ar.alloc([NT, D], F32)
            self.t_acc = [Tok() for _ in range(NT)]
            self.phase_merge(own)
            self.phase_moe(own, lambda i: self.xout[i * 128:(i + 1) * 128, :])
        self.b.barrier()
        self.es.close()
        return self.nc


def _common_inputs(inputs, core):
    bidx, hf = core // 2, core % 2
    x = np.asarray(inputs["x"], dtype=np.float32)
    ctx = np.asarray(inputs["ctx"], dtype=np.float32)
    xin = np.concatenate([x[bidx, hf * 2048:(hf + 1) * 2048], ctx[bidx, hf * 128:(hf + 1) * 128]], axis=0)
    cvec = np.stack([np.asarray(inputs["c"], np.float32)[bidx], np.asarray(inputs["c_ctx"], np.float32)], 0)
    cvecT = np.ascontiguousarray(cvec.reshape(2, 8, 128).transpose(2, 0, 1))
    n = hf * 2048 + np.arange(2048)
    rowpos = np.zeros((128, NT), np.float32)
    colpos = np.zeros((128, NT), np.float32)
    rowpos[:, :16] = (n // 64).reshape(16, 128).T
    colpos[:, :16] = (n % 64).reshape(16, 128).T
    return dict(xin=np.ascontiguousarray(xin), cvecT=cvecT, ident=np.eye(128, dtype=np.float32), rowpos=rowpos,
                colpos=colpos)


_PROGS = {}


def _prog(mode, layer):
    k = (mode, layer)
    if k not in _PROGS:
        _PROGS[k] = Prog(mode, layer).build()
    return _PROGS[k]


def _f(inputs, k, l=None):
    a = np.asarray(inputs[k], np.float32)
    if l is not None:
        a = a[l:l + 1]
    return np.ascontiguousarray(a)


def run_A(inputs, layer, xins):
    nc = _prog("A", layer)
    shared = {k: _f(inputs, k, layer) for k in ("g_attn", "w_in", "g_ckv", "w_ukv", "g_ka", "g_kb")}
    shared["w_mod"] = np.ascontiguousarray(np.asarray(inputs["w_mod"], np.float32)[layer:layer + 1, :, :2 * D])
    shared["b_mod"] = np.ascontiguousarray(np.asarray(inputs["b_mod"], np.float32)[layer:layer + 1, :2 * D])
    in_maps = []
    for c in range(NCORES):
        d = _common_inputs(inputs, c)
        d["xin"] = xins[c]
        d.update(shared)
        in_maps.append(d)
    res = run_bass_kernel_spmd(nc, in_maps, core_ids=list(range(NCORES)))
    return res.results


def assemble_kv(resA):
    out = []
    for bidx in range(NB):
        r0, r1 = resA[2 * bidx], resA[2 * bidx + 1]
        d = {}
        for k in ("KaT", "KbT"):
            a0, a1 = np.asarray(r0[k]), np.asarray(r1[k])
            d[k] = np.ascontiguousarray(np.concatenate([a0[..., 2048:], a1[..., 2048:], a0[..., :2048], a1[..., :2048]], -1))
        for k in ("Va", "Vb"):
            a0, a1 = np.asarray(r0[k]), np.asarray(r1[k])
            d[k] = np.ascontiguousarray(np.concatenate([a0[2048:], a1[2048:], a0[:2048], a1[:2048]], 0))
        out.append(d)
    return out


def run_B(inputs, layer, xins, kv):
    nc = _prog("B", layer)
    names = ("w_mod", "b_mod", "g_attn", "w_in", "g_ffn", "g_cq", "w_uq", "g_qa", "g_qb", "w_oa", "w_ob", "w_out",
             "w_e_gate", "w_e_up", "w_e_down", "w_s_gate", "w_s_up", "w_s_down")
    shared = {k: _f(inputs, k, layer) for k in names}
    shared["w_router"] = _f(inputs, "w_router")
    shared["b_router"] = _f(inputs, "b_router").reshape(1, 16)
    in_maps = []
    for c in range(NCORES):
        d = _common_inputs(inputs, c)
        d["xin"] = xins[c]
        d.update(shared)
        d.update(kv[c // 2])
        in_maps.append(d)
    res = run_bass_kernel_spmd(nc, in_maps, core_ids=list(range(NCORES)))
    return res.results


def _fused_inputs(inputs, core):
    bidx, hf = core // 2, core % 2
    x = np.asarray(inputs["x"], dtype=np.float32)
    ctx = np.asarray(inputs["ctx"], dtype=np.float32)
    own = slice(hf * 2048, (hf + 1) * 2048)
    oth = slice((1 - hf) * 2048, (2 - hf) * 2048)
    xin = np.concatenate([x[bidx, own], x[bidx, oth], ctx[bidx]], axis=0)
    cvec = np.stack([np.asarray(inputs["c"], np.float32)[bidx], np.asarray(inputs["c_ctx"], np.float32)], 0)
    cvecT = np.ascontiguousarray(cvec.reshape(2, 8, 128).transpose(2, 0, 1))
    n = np.concatenate([np.arange(4096)[own], np.arange(4096)[oth]])
    rowpos = np.zeros((128, 34), np.float32)
    colpos = np.zeros((128, 34), np.float32)
    rowpos[:, :32] = (n // 64).reshape(32, 128).T
    colpos[:, :32] = (n % 64).reshape(32, 128).T
    return dict(xin=np.ascontiguousarray(xin), cvecT=cvecT, ident=np.eye(128, dtype=np.float32), rowpos=rowpos,
                colpos=colpos)


def kernel_unfused(**inputs):
    xins = [_common_inputs(inputs, c)["xin"] for c in range(NCORES)]
    for layer in range(2):
        resA = run_A(inputs, layer, xins)
        kv = assemble_kv(resA)
        resB = run_B(inputs, layer, xins, kv)
        xins = [np.ascontiguousarray(np.asarray(resB[c]["xout"], np.float32)) for c in range(NCORES)]
    out = np.empty((NB, SEQ, D), np.float32)
    for c in range(NCORES):
        out[c // 2, (c % 2) * 2048:(c % 2 + 1) * 2048] = xins[c][:2048]
    return out


def kernel(**inputs):
    nc = _prog("F", 0)
    names = ("w_mod", "b_mod", "g_attn", "w_in", "g_ckv", "w_ukv", "g_ka", "g_kb", "g_ffn", "g_cq", "w_uq", "g_qa", "g_qb",
             "w_oa", "w_ob", "w_out", "w_e_gate", "w_e_up", "w_e_down", "w_s_gate", "w_s_up", "w_s_down", "w_router")
    shared = {k: _f(inputs, k) for k in names}
    shared["b_router"] = _f(inputs, "b_router").reshape(1, 16)
    in_maps = []
    for c in range(NCORES):
        d = _fused_inputs(inputs, c)
        d.update(shared)
        in_maps.append(d)
    res = run_bass_kernel_spmd(nc, in_maps, core_ids=list(range(NCORES)))
    out = np.empty((NB, SEQ, D), np.float32)
    for c in range(NCORES):
        out[c // 2, (c % 2) * 2048:(c % 2 + 1) * 2048] = np.asarray(res.results[c]["xout"], np.float32)
    return out
```

```python
import numpy as np
import ml_dtypes
from contextlib import ExitStack
import concourse.bass as bass
import concourse.mybir as mybir
from concourse.bass_utils import run_bass_kernel_spmd

F32 = mybir.dt.float32
BF16 = mybir.dt.bfloat16
I32 = mybir.dt.int32
AF = mybir.ActivationFunctionType
ALU = mybir.AluOpType
AX = mybir.AxisListType

D = 1024
NB = 4
SEQ = 4096
CTX = 256
NCORES = 8
NLAT_T = 16
NT = 17
NTOK = NT * 128
NKEY = SEQ + CTX
NKT = NKEY // 128
EPS = 1e-6
THETA = 10000.0
IN_COLS = 3232
C_CQ, C_CKV, C_KR, C_QB, C_KB, C_VB, C_GA = 0, 256, 384, 416, 928, 1056, 1184
SCALE_A = 96 ** -0.5
SCALE_B = 64 ** -0.5
PI = float(np.pi)


def _prod(s):
    r = 1
    for v in s:
        r *= v
    return r


class Tok:
    __slots__ = ("lw", "rs", "excl")

    def __init__(self, excl=False):
        self.lw = None
        self.rs = []
        self.excl = excl


class Bld:
    ROLL = 12000

    def __init__(self, nc, es):
        self.nc = nc
        self.es = es
        self.sems = []
        self.eng = {"pe": nc.tensor, "act": nc.scalar, "dve": nc.vector, "pool": nc.gpsimd, "sp": nc.sync}
        self.esem = {}
        self.ecnt = {}
        self.pe_sems = set()
        self.waited = {e: {} for e in self.eng}
        for e in self.eng:
            self._roll(e)
        self.dq = {}
        self.semval = {}
        for q, n in (("sp", 16), ("pool", 12), ("act", 4)):
            ids = [self._new_sem(f"d_{q}{i}") for i in range(n)]
            self.dq[q] = [ids, 0]
            for i in ids:
                self.semval[i] = 0
        self.nins = 0

    def _new_sem(self, name):
        h = self.es.enter_context(self.nc.semaphore(name))
        self.sems.append(h)
        return len(self.sems) - 1

    def _roll(self, e):
        s = self._new_sem(f"e_{e}{len(self.sems)}")
        self.esem[e] = s
        self.ecnt[e] = 0
        if e == "pe":
            self.pe_sems.add(s)

    def _deps(self, reads, writes):
        deps = set()
        for t in reads:
            if t.lw is not None:
                deps.add(t.lw)
        for t in writes:
            if t.lw is not None:
                deps.add(t.lw)
            deps.update(t.rs)
        return deps

    def _waits(self, e, deps):
        w = self.waited[e]
        best = {}
        for s, v in deps:
            if e == "pe" and s in self.pe_sems:
                continue
            if w.get(s, 0) >= v:
                continue
            if best.get(s, 0) < v:
                best[s] = v
        for s, v in best.items():
            w[s] = v
            self.eng[e].wait_ge(self.sems[s], v)

    def _update(self, ev, reads, writes):
        for t in writes:
            t.lw = ev
            t.rs = []
        for t in reads:
            if t not in writes:
                t.rs.append(ev)

    def op(self, e, fn, reads=(), writes=()):
        ex = [t for t in reads if t.excl and t not in writes]
        if ex:
            writes = list(writes) + ex
        if self.ecnt[e] >= self.ROLL:
            self._roll(e)
        self._waits(e, self._deps(reads, writes))
        self.ecnt[e] += 1
        ev = (self.esem[e], self.ecnt[e])
        fn(self.eng[e]).then_inc(self.sems[ev[0]], 1)
        self._update(ev, reads, writes)
        self.nins += 1
        return ev

    def dma(self, q, out, in_, reads=(), writes=()):
        ids, i = self.dq[q]
        s = ids[i % len(ids)]
        self.dq[q][1] = i + 1
        deps = self._deps(reads, writes)
        prev = self.semval[s]
        if prev > 0:
            deps.add((s, prev))
        self._waits(q, deps)
        self.semval[s] = prev + 16
        assert self.semval[s] < 30000
        ev = (s, prev + 16)
        self.eng[q].dma_start(out=out, in_=in_).then_inc(self.sems[s], 16)
        self._update(ev, reads, writes)
        self.nins += 1
        return ev

    def barrier(self):
        evs = set()
        for e in self.eng:
            if self.ecnt[e] > 0:
                evs.add((self.esem[e], self.ecnt[e]))
        for s, v in self.semval.items():
            if v > 0:
                evs.add((s, v))
        for e in self.eng:
            w = self.waited[e]
            for s, v in evs:
                if w.get(s, 0) >= v:
                    continue
                w[s] = v
                self.eng[e].wait_ge(self.sems[s], v)


class Arena:
    def __init__(self, t, nelem):
        self.t = t
        self.cap = nelem
        self.off = 0

    def alloc(self, free_shape, dtype):
        n = _prod(free_shape)
        nb = n * (4 if dtype in (F32, I32) else 2)
        nel = (nb + 63) // 64 * 32
        assert self.off + nel <= self.cap, f"arena overflow {self.off}+{nel}>{self.cap}"
        ap = self.t[:, self.off:self.off + nb // 2]
        self.off += nel
        if dtype != BF16:
            ap = ap.bitcast(dtype)
        if len(free_shape) == 2:
            ap = ap.rearrange("p (a b) -> p a b", a=free_shape[0], b=free_shape[1])
        elif len(free_shape) == 3:
            ap = ap.rearrange("p (a b c) -> p a b c", a=free_shape[0], b=free_shape[1], c=free_shape[2])
        return ap

    def mark(self):
        return self.off

    def release(self, m):
        self.off = m


def bc(ap, shape):
    return ap.broadcast_to(list(shape))


class Prog:
    def __init__(self, mode, layer):
        self.mode = mode
        self.l = 0
        self.last = layer == 1
        if mode == "F":
            self.NT, self.ctx_tiles, self.ctx_keys = 34, {32, 33}, [32, 33]
        else:
            self.NT, self.ctx_tiles, self.ctx_keys = 17, {16}, [0, 1]
        self.NTOK = self.NT * 128
        nl = 2 if mode == "F" else 1
        nc = bass.Bass("TRN2", target_bir_lowering=False)
        self.nc = nc
        self.es = ExitStack()
        es = self.es
        dt = nc.dram_tensor

        def inp(name, shape, dtype=F32):
            return dt(name, list(shape), dtype, kind="ExternalInput").ap()

        def outp(name, shape, dtype=F32):
            return dt(name, list(shape), dtype, kind="ExternalOutput").ap()

        self.inp = inp
        self.outp = outp
        self.xin = inp("xin", [self.NTOK, D])
        self.xsrc = self.xin
        self.cvecT = inp("cvecT", [128, 2, 8])
        self.ident = inp("ident", [128, 128])
        self.rowpos = inp("rowpos", [128, self.NT])
        self.colpos = inp("colpos", [128, self.NT])
        nseg = 2 if mode == "A" else 6
        self.w_mod = inp("w_mod", [nl, D, nseg * D])
        self.b_mod = inp("b_mod", [nl, nseg * D])
        self.g_attn = inp("g_attn", [nl, D])
        self.w_in = inp("w_in", [nl, D, IN_COLS])
        if mode in ("A", "F"):
            self.g_ckv = inp("g_ckv", [nl, 128])
            self.w_ukv = inp("w_ukv", [nl, 128, 1024])
            self.g_ka = inp("g_ka", [nl, 96])
            self.g_kb = inp("g_kb", [nl, 64])
        if mode == "A":
            self.KaT = outp("KaT", [8, 96, NTOK], BF16)
            self.KbT = outp("KbT", [2, 64, NTOK], BF16)
            self.Va = outp("Va", [NTOK, 8, 64], BF16)
            self.Vb = outp("Vb", [NTOK, 2, 64], BF16)
        if mode in ("B", "F"):
            self.g_ffn = inp("g_ffn", [nl, D])
            self.g_cq = inp("g_cq", [nl, 256])
            self.w_uq = inp("w_uq", [nl, 256, 768])
            self.g_qa = inp("g_qa", [nl, 96])
            self.g_qb = inp("g_qb", [nl, 64])
            self.w_oa = inp("w_oa", [nl, 512, D])
            self.w_ob = inp("w_ob", [nl, 512, D])
            self.w_out = inp("w_out", [nl, D, D])
            self.w_router = inp("w_router", [D, 16])
            self.b_router = inp("b_router", [1, 16])
            self.w_e_gate = inp("w_e_gate", [nl, 16, D, 512])
            self.w_e_up = inp("w_e_up", [nl, 16, D, 512])
            self.w_e_down = inp("w_e_down", [nl, 16, 512, D])
            self.w_s_gate = inp("w_s_gate", [nl, D, 512])
            self.w_s_up = inp("w_s_up", [nl, D, 512])
            self.w_s_down = inp("w_s_down", [nl, 512, D])
            if mode == "B":
                self.KaT = inp("KaT", [8, 96, NKEY], BF16)
                self.KbT = inp("KbT", [2, 64, NKEY], BF16)
                self.Va = inp("Va", [NKEY, 8, 64], BF16)
                self.Vb = inp("Vb", [NKEY, 2, 64], BF16)
                self.xout = outp("xout", [NTOK, D])
            else:
                self.KaT = dt("KaT", [8, 96, NKEY], BF16).ap()
                self.KbT = dt("KbT", [2, 64, NKEY], BF16).ap()
                self.Va = dt("Va", [128, NKT, 8 * 66], BF16).ap()
                self.Vb = dt("Vb", [128, NKT, 2 * 66], BF16).ap()
                self.xs1 = dt("xs1", [NKEY, D], F32).ap()
                self.xout = outp("xout", [2048, D])
            self.QaT = dt("QaT", [8, 96, self.NTOK], BF16).ap()
            self.QbT = dt("QbT", [8, 64, self.NTOK], BF16).ap()
            self.Gs = dt("Gs", [self.NTOK, 2048], BF16).ap()
            self.OT = dt("OT", [16, 64, self.NTOK], BF16).ap()
        arena_t = es.enter_context(nc.sbuf_tensor("arena", [128, 106000], BF16))
        self.ar = Arena(arena_t, 106000)
        psum_t = es.enter_context(nc.psum_tensor("psum", [128, 4096], F32))
        self.psum = psum_t
        self.b = Bld(nc, es)
        self.ptok = [Tok(excl=True) for _ in range(8)]

    def bank(self, i, n=1):
        return self.psum[:, i * 512:(i + n) * 512]

    def bank_bf(self, i):
        return self.psum[:, i * 512:(i + 1) * 512].bitcast(BF16)

    def load_bcast(self, dst, src_row):
        t = Tok()
        self.b.dma("sp", dst, src_row.partition_broadcast(128), writes=[t])
        return t

    def setup_consts(self):
        b, ar = self.b, self.ar
        self.identf = ar.alloc([128], F32)
        self.identb = ar.alloc([128], BF16)
        self.t_identf = Tok()
        self.t_identb = Tok()
        b.dma("sp", self.identf, self.ident, writes=[self.t_identf])
        b.dma("pool", self.identb, self.ident, writes=[self.t_identb])
        self.onesf = ar.alloc([64], F32)
        self.t_ones = Tok()
        b.op("dve", lambda e: e.memset(self.onesf, 1.0), writes=[self.t_ones])

    def setup_rope(self):
        b, ar = self.b, self.ar
        NT = self.NT
        rp = ar.alloc([NT], F32)
        cp = ar.alloc([NT], F32)
        t_pos = Tok()
        b.dma("sp", rp, self.rowpos, writes=[t_pos])
        b.dma("sp", cp, self.colpos, writes=[t_pos])
        self.rope = {}
        for name, half in (("a", 8), ("b", 16)):
            R = 4 * half
            C = ar.alloc([NT, R], F32)
            S = ar.alloc([NT, R], F32)
            tC = Tok()
            m = ar.mark()
            inv = ar.alloc([half], F32)
            t_inv = Tok()
            for j in range(half):
                val = float(THETA ** (-(2.0 * j) / (2 * half)))
                b.op("dve", lambda e, j=j, val=val: e.memset(inv[:, j:j + 1], val), writes=[t_inv])
            ang = ar.alloc([NT, half], F32)
            tmpf = ar.alloc([NT, half], F32)
            tmpi = ar.alloc([NT, half], I32)
            red = ar.alloc([NT, half], F32)
            t_ang, t_f, t_i, t_r = Tok(), Tok(), Tok(), Tok()
            for ci, pos in enumerate((rp, cp)):
                b.op("dve", lambda e, pos=pos: e.tensor_tensor(
                    out=ang, in0=bc(pos.unsqueeze(2), [128, NT, half]), in1=bc(inv.unsqueeze(1), [128, NT, half]),
                    op=ALU.mult), reads=[t_pos, t_inv], writes=[t_ang])
                for which, shift in (("sin", 0.0), ("cos", PI / 2)):
                    b.op("dve", lambda e, shift=shift: e.tensor_scalar(
                        out=tmpf, in0=ang, scalar1=shift, scalar2=1.0 / (2 * PI), op0=ALU.add, op1=ALU.mult),
                        reads=[t_ang], writes=[t_f])
                    b.op("dve", lambda e: e.tensor_copy(out=tmpi, in_=tmpf), reads=[t_f], writes=[t_i])
                    b.op("dve", lambda e: e.tensor_copy(out=tmpf, in_=tmpi), reads=[t_i], writes=[t_f])
                    b.op("dve", lambda e: e.scalar_tensor_tensor(
                        out=red, in0=tmpf, scalar=-2 * PI, in1=ang, op0=ALU.mult, op1=ALU.add),
                        reads=[t_f, t_ang], writes=[t_r])
                    b.op("dve", lambda e, shift=shift: e.tensor_scalar(
                        out=red, in0=red, scalar1=shift, scalar2=PI, op0=ALU.add, op1=ALU.min),
                        reads=[t_r], writes=[t_r])
                    b.op("dve", lambda e: e.tensor_scalar(
                        out=red, in0=red, scalar1=-PI, scalar2=None, op0=ALU.max), reads=[t_r], writes=[t_r])
                    base = ci * 2 * half
                    if which == "sin":
                        b.op("act", lambda e, base=base: e.activation(
                            out=S[:, :, base + half:base + 2 * half], in_=red, func=AF.Sin), reads=[t_r], writes=[tC])
                        b.op("dve", lambda e, base=base: e.tensor_scalar(
                            out=S[:, :, base:base + half], in0=S[:, :, base + half:base + 2 * half], scalar1=-1.0,
                            scalar2=None, op0=ALU.mult), reads=[tC], writes=[tC])
                    else:
                        b.op("act", lambda e, base=base: e.activation(
                            out=C[:, :, base:base + half], in_=red, func=AF.Sin), reads=[t_r], writes=[tC])
                        b.op("dve", lambda e, base=base: e.tensor_copy(
                            out=C[:, :, base + half:base + 2 * half], in_=C[:, :, base:base + half]),
                            reads=[tC], writes=[tC])
            self.b.barrier()
            ar.release(m)
            self.rope[name] = (C, S, tC, half)

    def emit_mod(self, segs):
        b, ar, l = self.b, self.ar, self.l
        res = {}
        for s in (0, 1):
            for seg in segs:
                res[(s, seg)] = (ar.alloc([D], F32), Tok())
        m = ar.mark()
        cT = ar.alloc([2, 8], F32)
        sc = ar.alloc([2, 8], F32)
        scb = ar.alloc([2, 8, 128], BF16)
        t_c, t_sc, t_scb = Tok(), Tok(), Tok()
        b.dma("sp", cT, self.cvecT, writes=[t_c])
        b.op("act", lambda e: e.activation(out=sc, in_=cT, func=AF.Silu), reads=[t_c], writes=[t_sc])
        for s in (0, 1):
            b.op("dve", lambda e, s=s: e.tensor_copy(out=scb[:, s], in_=bc(sc[:, s].unsqueeze(2), [128, 8, 128])),
                 reads=[t_sc], writes=[t_scb])
        wm = [ar.alloc([8, 512], BF16) for _ in range(4)]
        t_wm = [Tok() for _ in range(4)]
        bm = [ar.alloc([512], F32) for _ in range(4)]
        t_bm = [Tok() for _ in range(4)]
        k = 0
        for seg in segs:
            for j in range(2):
                c0 = seg * D + j * 512
                buf = k % 4
                k += 1
                b.dma("pool", wm[buf], self.w_mod[l, :, c0:c0 + 512].rearrange("(k p) c -> p k c", p=128),
                      writes=[t_wm[buf]])
                b.dma("sp", bm[buf], self.b_mod[l:l + 1, c0:c0 + 512].partition_broadcast(128), writes=[t_bm[buf]])
                for s in (0, 1):
                    pb = 2 * buf + s
                    for kk in range(8):
                        b.op("pe", lambda e, kk=kk, s=s, buf=buf, pb=pb: e.matmul(
                            self.bank(pb), lhsT=scb[:, s, kk, :], rhs=wm[buf][:, kk, :], start=(kk == 0), stop=(kk == 7)),
                            reads=[t_scb, t_wm[buf]], writes=[self.ptok[pb]])
                    dst, tk = res[(s, seg)]
                    b.op("dve", lambda e, dst=dst, j=j, pb=pb, buf=buf: e.tensor_tensor(
                        out=dst[:, j * 512:(j + 1) * 512], in0=self.bank(pb), in1=bm[buf], op=ALU.add),
                        reads=[self.ptok[pb], t_bm[buf]], writes=[tk])
        b.barrier()
        ar.release(m)
        return res

    def run_il(self, gens, depth=2):
        it = iter(gens)
        active = []
        while True:
            while len(active) < depth:
                g = next(it, None)
                if g is None:
                    break
                active.append(g)
            if not active:
                break
            for g in list(active):
                try:
                    next(g)
                except StopIteration:
                    active.remove(g)

    def rstd_from_ss(self, ss, n_h, denom, t_ss):
        b = self.b
        b.op("dve", lambda e: e.tensor_scalar(out=ss, in0=ss, scalar1=1.0 / denom, scalar2=EPS, op0=ALU.mult,
                                              op1=ALU.add), reads=[t_ss], writes=[t_ss])
        yield
        b.op("act", lambda e: e.activation(out=ss, in_=ss, func=AF.Sqrt), reads=[t_ss], writes=[t_ss])
        yield
        b.op("dve", lambda e: e.reciprocal(out=ss, in_=ss), reads=[t_ss], writes=[t_ss])
        yield

    def norm_mod_tile(self, xt, t_x, gm, sh, hout, t_h, scratch, t_scr, st, t_st):
        b = self.b
        b.op("act", lambda e: e.activation(out=scratch, in_=xt, func=AF.Square, accum_out=st),
             reads=[t_x], writes=[t_scr, t_st])
        yield
        yield from self.rstd_from_ss(st, 1, D, t_st)
        b.op("dve", lambda e: e.scalar_tensor_tensor(out=scratch, in0=xt, scalar=st, in1=gm[0], op0=ALU.mult,
                                                     op1=ALU.mult), reads=[t_x, t_st, gm[1]], writes=[t_scr])
        yield
        b.op("dve", lambda e: e.tensor_tensor(out=hout, in0=scratch, in1=sh[0], op=ALU.add),
             reads=[t_scr, sh[1]], writes=[t_h])
        yield

    def transpose_rows(self, src, t_src, nchunk, csize, pbank, dst, t_dst, ident=None, evac="act"):
        b = self.b
        pv = self.bank_bf(pbank)
        for c in range(nchunk):
            b.op("pe", lambda e, c=c: e.transpose(pv[0:csize, c * 128:(c + 1) * 128], src[:, c, :], self.identb),
                 reads=[t_src, self.t_identb], writes=[self.ptok[pbank]])
        yield
        srcv = pv[0:csize, 0:nchunk * 128].rearrange("p (a b) -> p a b", a=nchunk, b=128)
        if evac == "act":
            b.op("act", lambda e: e.activation(out=dst, in_=srcv, func=AF.Copy), reads=[self.ptok[pbank]], writes=[t_dst])
        else:
            b.op("dve", lambda e: e.tensor_copy(out=dst, in_=srcv), reads=[self.ptok[pbank]], writes=[t_dst])
        yield

    def head_norm(self, src, t_src, H, Dh, extra_ss, gvec, t_g, sq, t_sq, ss, t_ss, dst_f, t_dstf):
        b = self.b
        b.op("act", lambda e: e.activation(out=sq, in_=src, func=AF.Square), reads=[t_src], writes=[t_sq])
        yield
        b.op("dve", lambda e: e.tensor_reduce(out=ss, in_=sq, axis=AX.X, op=ALU.add), reads=[t_sq], writes=[t_ss])
        yield
        denom = Dh
        if extra_ss is not None:
            ex, t_ex, n_ex = extra_ss
            b.op("dve", lambda e: e.tensor_tensor(out=ss, in0=ss, in1=bc(ex, [128, H]), op=ALU.add),
                 reads=[t_ss, t_ex], writes=[t_ss])
            yield
            denom = Dh + n_ex
        yield from self.rstd_from_ss(ss, H, denom, t_ss)
        b.op("dve", lambda e: e.tensor_tensor(out=dst_f, in0=src, in1=bc(ss.unsqueeze(2), [128, H, Dh]), op=ALU.mult),
             reads=[t_src, t_ss], writes=[t_dstf])
        yield
        b.op("dve", lambda e: e.tensor_tensor(out=dst_f, in0=dst_f, in1=bc(gvec.unsqueeze(1), [128, H, Dh]),
                                              op=ALU.mult), reads=[t_dstf, t_g], writes=[t_dstf])
        yield

    def rope_apply(self, v, t_v, H, which, tile, dst, t_dst, t1, t2, t_t):
        b = self.b
        C, S, tC, half = self.rope[which]
        R = 4 * half
        Ct = C[:, tile, :]
        St = S[:, tile, :]
        b.op("dve", lambda e: e.tensor_tensor(out=t1, in0=v, in1=bc(Ct.unsqueeze(1), [128, H, R]), op=ALU.mult),
             reads=[t_v, tC], writes=[t_t])
        v4 = v.rearrange("p h (c t j) -> p h c t j", c=2, t=2, j=half)
        t24 = t2.rearrange("p h (c t j) -> p h c t j", c=2, t=2, j=half)
        S4 = St.rearrange("p (c t j) -> p c t j", c=2, t=2, j=half)
        for tt in range(2):
            b.op("dve", lambda e, tt=tt: e.tensor_tensor(
                out=t24[:, :, :, tt, :], in0=v4[:, :, :, 1 - tt, :],
                in1=bc(S4[:, :, tt, :].unsqueeze(1), [128, H, 2, half]), op=ALU.mult),
                reads=[t_v, tC], writes=[t_t])
        yield
        b.op("dve", lambda e: e.tensor_tensor(out=dst, in0=t1, in1=t2, op=ALU.add), reads=[t_t], writes=[t_dst])
        yield

    def _common_setup(self):
        b, ar, l = self.b, self.ar, self.l
        self.setup_rope()
        mod = self.emit_mod([0, 1])
        gat = ar.alloc([D], F32)
        t_gat = self.load_bcast(gat, self.g_attn[l:l + 1, :])
        gm = {}
        for s in (0, 1):
            ap, tk = mod[(s, 1)]
            b.op("dve", lambda e, ap=ap: e.scalar_tensor_tensor(out=ap, in0=ap, scalar=1.0, in1=gat, op0=ALU.add,
                                                                op1=ALU.mult), reads=[tk, t_gat], writes=[tk])
            gm[s] = (ap, tk)
        return mod, gm

    def _q_chain(self, u, ti, pb, W):
        b = self.b
        PB_CQ, PB_QB, PB_CT, PB_QA, PB_QTA, PB_QTB = pb + 1, pb + 2, pb, pb + 2, pb + 2, pb + 3
        for kk in range(8):
            b.op("pe", lambda e, kk=kk: e.matmul(self.bank(PB_CQ)[:, 0:256], lhsT=u.hT[:, kk, :], rhs=W.wq1[:, kk, :],
                                                 start=(kk == 0), stop=(kk == 7)),
                 reads=[u.t_hT, W.t_w], writes=[self.ptok[PB_CQ]])
        for kk in range(8):
            b.op("pe", lambda e, kk=kk: e.matmul(self.bank(PB_QB), lhsT=u.hT[:, kk, :], rhs=W.wq2[:, kk, :],
                                                 start=(kk == 0), stop=(kk == 7)),
                 reads=[u.t_hT, W.t_w], writes=[self.ptok[PB_QB]])
        yield
        cq = self.bank(PB_CQ)[:, 0:256]
        b.op("act", lambda e: e.activation(out=u.junk, in_=cq, func=AF.Square, accum_out=u.st[:, 1:2]),
             reads=[self.ptok[PB_CQ]], writes=[u.t_junk, u.t_st])
        qbp = self.bank(PB_QB).rearrange("p (h d) -> p h d", h=8, d=64)
        b.op("act", lambda e: e.activation(out=u.qbf, in_=qbp, func=AF.Copy), reads=[self.ptok[PB_QB]],
             writes=[u.t_qbf])
        yield
        yield from self.rstd_from_ss(u.st[:, 1:2], 1, 256, u.t_st)
        b.op("dve", lambda e: e.scalar_tensor_tensor(out=u.cqn.rearrange("p a b -> p (a b)"), in0=cq,
                                                     scalar=u.st[:, 1:2], in1=W.gcq, op0=ALU.mult, op1=ALU.mult),
             reads=[self.ptok[PB_CQ], u.t_st, W.t_g], writes=[u.t_cqn])
        yield
        yield from self.transpose_rows(u.cqn, u.t_cqn, 2, 128, PB_CT, u.cqnT, u.t_cqnT, evac="dve")
        for j, (c0, c1) in enumerate(((0, 512), (512, 768))):
            for kk in range(2):
                b.op("pe", lambda e, j=j, kk=kk, c0=c0, c1=c1: e.matmul(
                    self.bank(PB_QA + j)[:, 0:c1 - c0], lhsT=u.cqnT[:, kk, :], rhs=W.wuq[:, kk, c0:c1],
                    start=(kk == 0), stop=(kk == 1)), reads=[u.t_cqnT, W.t_w], writes=[self.ptok[PB_QA + j]])
        for j in range(4):
            pbg = (PB_CQ, PB_CT, PB_QA, PB_QA + 1)[j]
            for kk in range(8):
                b.op("pe", lambda e, kk=kk, j=j, pb=pbg: e.matmul(self.bank(pb), lhsT=u.hT[:, kk, :],
                                                                 rhs=W.wg[:, kk, j * 512:(j + 1) * 512],
                                                                 start=(kk == 0), stop=(kk == 7)),
                     reads=[u.t_hT, W.t_w], writes=[self.ptok[pbg]])
            b.op("act", lambda e, j=j, pb=pbg: e.activation(out=u.Gsb[:, j * 512:(j + 1) * 512], in_=self.bank(pb),
                                                            func=AF.Sigmoid), reads=[self.ptok[pbg]], writes=[u.t_Gsb])
            if j == 0:
                qa = self.bank(PB_QA, 2)[:, 0:768].rearrange("p (h d) -> p h d", h=8, d=96)
                b.op("act", lambda e: e.activation(out=u.qaf, in_=qa, func=AF.Copy),
                     reads=[self.ptok[PB_QA], self.ptok[PB_QA + 1]], writes=[u.t_qaf])
            yield
        b.dma("sp", self.Gs[ti * 128:(ti + 1) * 128, :], u.Gsb, reads=[u.t_Gsb], writes=[W.t_out])
        yield from self.head_norm(u.qaf, u.t_qaf, 8, 96, None, W.gqa, W.t_g2, u.sq, u.t_sq, u.ss8, u.t_ss8, u.qaf, u.t_qaf)
        b.op("act", lambda e: e.activation(out=u.Qab[:, :, 0:64], in_=u.qaf[:, :, 0:64], func=AF.Copy),
             reads=[u.t_qaf], writes=[u.t_Qab])
        yield from self.rope_apply(u.qaf[:, :, 64:96], u.t_qaf, 8, "a", ti, u.Qab[:, :, 64:96], u.t_Qab,
                                   u.r1[:, :, 0:32], u.r2[:, :, 0:32], u.t_r1)
        yield from self.transpose_rows(u.Qab, u.t_Qab, 8, 96, PB_QTA, u.QaTs[0:96], u.t_QaTs)
        b.dma("sp", self.QaT[:, :, ti * 128:(ti + 1) * 128].rearrange("h d t -> d h t"), u.QaTs[0:96],
              reads=[u.t_QaTs], writes=[W.t_out])
        yield from self.head_norm(u.qbf, u.t_qbf, 8, 64, None, W.gqb, W.t_g3, u.sq[:, :, 0:64], u.t_sq, u.ss8b, u.t_ss8b,
                                  u.qbf, u.t_qbf)
        yield from self.rope_apply(u.qbf, u.t_qbf, 8, "b", ti, u.Qbb, u.t_Qbb, u.r1, u.r2, u.t_r1)
        yield from self.transpose_rows(u.Qbb, u.t_Qbb, 8, 64, PB_QTB, u.QbTs[0:64], u.t_QbTs, evac="dve")
        b.dma("sp", self.QbT[:, :, ti * 128:(ti + 1) * 128].rearrange("h d t -> d h t"), u.QbTs[0:64],
              reads=[u.t_QbTs], writes=[W.t_out])
        yield

    def phase_kvq(self, tiles, qtiles, setup):
        from types import SimpleNamespace as NS
        b, ar, l = self.b, self.ar, self.l
        m0 = ar.mark()
        mod, gm = setup
        wkv1 = ar.alloc([8, 160], BF16)
        wkv2 = ar.alloc([8, 256], BF16)
        wukv = ar.alloc([1024], BF16)
        t_w = Tok()
        wv = self.w_in[l].rearrange("(k p) c -> p k c", p=128)
        b.dma("pool", wkv1, wv[:, :, C_CKV:C_QB], writes=[t_w])
        b.dma("pool", wkv2, wv[:, :, C_KB:C_GA], writes=[t_w])
        b.dma("pool", wukv, self.w_ukv[l], writes=[t_w])
        gckv = ar.alloc([128], F32)
        gka = ar.alloc([96], F32)
        gkb = ar.alloc([64], F32)
        t_g = self.load_bcast(gckv, self.g_ckv[l:l + 1, :])
        t_g2 = self.load_bcast(gka, self.g_ka[l:l + 1, :])
        t_g3 = self.load_bcast(gkb, self.g_kb[l:l + 1, :])

        W = NS()
        W.wq1 = ar.alloc([8, 256], BF16)
        W.wq2 = ar.alloc([8, 512], BF16)
        W.wg = ar.alloc([8, 2048], BF16)
        W.wuq = ar.alloc([2, 768], BF16)
        W.t_w = Tok()
        b.dma("pool", W.wq1, wv[:, :, C_CQ:C_CKV], writes=[W.t_w])
        b.dma("pool", W.wq2, wv[:, :, C_QB:C_KB], writes=[W.t_w])
        for j in range(4):
            b.dma("pool", W.wg[:, :, j * 512:(j + 1) * 512], wv[:, :, C_GA + j * 512:C_GA + (j + 1) * 512],
                  writes=[W.t_w])
        b.dma("pool", W.wuq, self.w_uq[l].rearrange("(k p) c -> p k c", p=128), writes=[W.t_w])
        W.gcq = ar.alloc([256], F32)
        W.gqa = ar.alloc([96], F32)
        W.gqb = ar.alloc([64], F32)
        W.t_g = self.load_bcast(W.gcq, self.g_cq[l:l + 1, :])
        W.t_g2 = self.load_bcast(W.gqa, self.g_qa[l:l + 1, :])
        W.t_g3 = self.load_bcast(W.gqb, self.g_qb[l:l + 1, :])
        W.t_out = Tok()
        qset = set(qtiles)

        def mk():
            u = NS()
            for name, shp, dt_ in (("cqn", [2, 128], BF16), ("cqnT", [2, 128], BF16), ("ss8b", [8], F32),
                                   ("qaf", [8, 96], F32), ("qbf", [8, 64], F32), ("Qab", [8, 96], BF16),
                                   ("Qbb", [8, 64], BF16), ("QaTs", [8, 128], BF16), ("QbTs", [8, 128], BF16),
                                   ("Gsb", [2048], BF16), ("scr", [D], F32), ("st", [8], F32), ("h", [8, 128], BF16),
                                   ("hT", [8, 128], BF16), ("ckvn", [1, 128], BF16), ("ckvnT", [1, 128], BF16),
                                   ("ss8", [8], F32), ("ss2", [2], F32), ("sq", [8, 96], F32), ("kaf", [8, 64], F32),
                                   ("krf", [1, 32], F32), ("krr", [1, 32], F32), ("r1", [8, 64], F32),
                                   ("r2", [8, 64], F32), ("Kab", [8, 96], BF16), ("KaTs", [8, 128], BF16),
                                   ("Vab", [8, 66], BF16), ("kbf", [2, 64], F32), ("Kbb", [2, 64], BF16),
                                   ("KbTs", [2, 128], BF16), ("Vbb", [2, 66], BF16), ("sskr", [1], F32),
                                   ("junk", [256], F32)):
                setattr(u, name, ar.alloc(shp, dt_))
                setattr(u, "t_" + name, Tok())
            u.sq64, u.t_sq64 = u.sq[:, :, 0:64], u.t_sq
            u.junk128, u.t_junk128 = u.junk[:, 0:128], u.t_junk
            return u
        sets = [mk(), mk()]
        for u_ in sets:
            b.op("dve", lambda e, u_=u_: e.memset(u_.Vab[:, :, 64:66], 1.0), writes=[u_.t_Vab])
            b.op("dve", lambda e, u_=u_: e.memset(u_.Vbb[:, :, 64:66], 1.0), writes=[u_.t_Vbb])
        t_out = Tok()
        xts = [ar.alloc([D], F32) for _ in range(4)]
        t_xts = [Tok() for _ in range(4)]

        def issue(j):
            if j < len(tiles):
                tj = tiles[j]
                b.dma("sp", xts[j % 4], self.xsrc[tj * 128:(tj + 1) * 128, :], writes=[t_xts[j % 4]])
        issue(0)
        issue(1)

        def tile(idx, ti):
            u = sets[idx % 2]
            u.xt, u.t_xt = xts[idx % 4], t_xts[idx % 4]
            issue(idx + 2)
            pb = 4 * (idx % 2)
            PB_HT, PB_KV1, PB_KV2, PB_CT, PB_KVA, PB_KAT, PB_KBT = pb, pb + 1, pb + 2, pb, pb + 2, pb, pb + 1
            s = 1 if ti in self.ctx_tiles else 0
            yield from self.norm_mod_tile(u.xt, u.t_xt, gm[s], mod[(s, 0)], u.h.rearrange("p a b -> p (a b)"), u.t_h,
                                          u.scr, u.t_scr, u.st[:, 0:1], u.t_st)
            yield from self.transpose_rows(u.h, u.t_h, 8, 128, PB_HT, u.hT, u.t_hT)
            for kk in range(8):
                b.op("pe", lambda e, kk=kk: e.matmul(self.bank(PB_KV1)[:, 0:160], lhsT=u.hT[:, kk, :], rhs=wkv1[:, kk, :],
                                                     start=(kk == 0), stop=(kk == 7)),
                     reads=[u.t_hT, t_w], writes=[self.ptok[PB_KV1]])
            for kk in range(8):
                b.op("pe", lambda e, kk=kk: e.matmul(self.bank(PB_KV2)[:, 0:256], lhsT=u.hT[:, kk, :], rhs=wkv2[:, kk, :],
                                                     start=(kk == 0), stop=(kk == 7)),
                     reads=[u.t_hT, t_w], writes=[self.ptok[PB_KV2]])
            yield
            ckv = self.bank(PB_KV1)[:, 0:128]
            kr = self.bank(PB_KV1)[:, 128:160]
            b.op("act", lambda e: e.activation(out=u.junk128, in_=ckv, func=AF.Square, accum_out=u.st[:, 1:2]),
                 reads=[self.ptok[PB_KV1]], writes=[u.t_junk, u.t_st])
            yield
            yield from self.rstd_from_ss(u.st[:, 1:2], 1, 128, u.t_st)
            b.op("dve", lambda e: e.scalar_tensor_tensor(out=u.ckvn[:, 0, :], in0=ckv, scalar=u.st[:, 1:2], in1=gckv,
                                                         op0=ALU.mult, op1=ALU.mult),
                 reads=[self.ptok[PB_KV1], u.t_st, t_g], writes=[u.t_ckvn])
            yield
            kbp = self.bank(PB_KV2)[:, 0:128].rearrange("p (h d) -> p h d", h=2, d=64)
            vbp = self.bank(PB_KV2)[:, 128:256].rearrange("p (h d) -> p h d", h=2, d=64)
            b.op("act", lambda e: e.activation(out=u.Vbb[:, :, 0:64], in_=vbp, func=AF.Copy), reads=[self.ptok[PB_KV2]],
                 writes=[u.t_Vbb])
            b.op("act", lambda e: e.activation(out=u.kbf, in_=kbp, func=AF.Copy), reads=[self.ptok[PB_KV2]],
                 writes=[u.t_kbf])
            yield
            b.dma("sp", self.Vb[:, ti, :], u.Vbb.rearrange("p a b -> p (a b)"), reads=[u.t_Vbb], writes=[t_out])
            b.op("act", lambda e: e.activation(out=u.junk128[:, 0:32], in_=kr, func=AF.Square, accum_out=u.sskr),
                 reads=[self.ptok[PB_KV1]], writes=[u.t_junk, u.t_sskr])
            b.op("dve", lambda e: e.tensor_tensor(out=u.krf[:, 0, :], in0=kr, in1=gka[:, 64:96], op=ALU.mult),
                 reads=[self.ptok[PB_KV1], t_g2], writes=[u.t_krf])
            yield
            yield from self.transpose_rows(u.ckvn, u.t_ckvn, 1, 128, PB_CT, u.ckvnT, u.t_ckvnT, evac="dve")
            for j in range(2):
                b.op("pe", lambda e, j=j: e.matmul(self.bank(PB_KVA + j), lhsT=u.ckvnT[:, 0, :],
                                                   rhs=wukv[:, j * 512:(j + 1) * 512], start=True, stop=True),
                     reads=[u.t_ckvnT, t_w], writes=[self.ptok[PB_KVA + j]])
            yield
            kva = self.bank(PB_KVA, 2).rearrange("p (h d) -> p h d", h=8, d=128)
            t_kva = [self.ptok[PB_KVA], self.ptok[PB_KVA + 1]]
            knope = kva[:, :, 0:64]
            yield from self.rope_apply(u.krf, u.t_krf, 1, "a", ti, u.krr, u.t_krr, u.r1[:, 0:1, 0:32], u.r2[:, 0:1, 0:32],
                                       u.t_r1)
            b.op("act", lambda e: e.activation(out=u.kaf, in_=knope, func=AF.Copy), reads=t_kva, writes=[u.t_kaf])
            b.op("act", lambda e: e.activation(out=u.Vab[:, :, 0:64], in_=kva[:, :, 64:128], func=AF.Copy), reads=t_kva,
                 writes=[u.t_Vab])
            yield
            b.dma("sp", self.Va[:, ti, :], u.Vab.rearrange("p a b -> p (a b)"), reads=[u.t_Vab], writes=[t_out])
            b.op("act", lambda e: e.activation(out=u.sq64, in_=u.kaf, func=AF.Square), reads=[u.t_kaf], writes=[u.t_sq])
            yield
            b.op("dve", lambda e: e.tensor_reduce(out=u.ss8, in_=u.sq64, axis=AX.X, op=ALU.add), reads=[u.t_sq],
                 writes=[u.t_ss8])
            yield
            b.op("dve", lambda e: e.tensor_tensor(out=u.ss8, in0=u.ss8, in1=bc(u.sskr, [128, 8]), op=ALU.add),
                 reads=[u.t_ss8, u.t_sskr], writes=[u.t_ss8])
            yield
            yield from self.rstd_from_ss(u.ss8, 8, 96, u.t_ss8)
            b.op("dve", lambda e: e.tensor_tensor(out=u.kaf, in0=u.kaf, in1=bc(u.ss8.unsqueeze(2), [128, 8, 64]),
                                                  op=ALU.mult), reads=[u.t_kaf, u.t_ss8], writes=[u.t_kaf])
            yield
            b.op("dve", lambda e: e.tensor_tensor(out=u.Kab[:, :, 0:64], in0=u.kaf,
                                                  in1=bc(gka[:, 0:64].unsqueeze(1), [128, 8, 64]), op=ALU.mult),
                 reads=[u.t_kaf, t_g2], writes=[u.t_Kab])
            b.op("dve", lambda e: e.tensor_tensor(out=u.Kab[:, :, 64:96], in0=bc(u.krr, [128, 8, 32]),
                                                  in1=bc(u.ss8.unsqueeze(2), [128, 8, 32]), op=ALU.mult),
                 reads=[u.t_krr, u.t_ss8], writes=[u.t_Kab])
            yield
            yield from self.transpose_rows(u.Kab, u.t_Kab, 8, 96, PB_KAT, u.KaTs[0:96], u.t_KaTs)
            b.dma("sp", self.KaT[:, :, ti * 128:(ti + 1) * 128].rearrange("h d t -> d h t"), u.KaTs[0:96],
                  reads=[u.t_KaTs], writes=[t_out])
            yield from self.head_norm(u.kbf, u.t_kbf, 2, 64, None, gkb, t_g3, u.sq64[:, 0:2, :], u.t_sq, u.ss2, u.t_ss2,
                                      u.kbf, u.t_kbf)
            yield from self.rope_apply(u.kbf, u.t_kbf, 2, "b", ti, u.Kbb, u.t_Kbb, u.r1[:, 0:2, :], u.r2[:, 0:2, :], u.t_r1)
            yield from self.transpose_rows(u.Kbb, u.t_Kbb, 2, 64, PB_KBT, u.KbTs[0:64], u.t_KbTs, evac="dve")
            b.dma("sp", self.KbT[:, :, ti * 128:(ti + 1) * 128].rearrange("h d t -> d h t"), u.KbTs[0:64],
                  reads=[u.t_KbTs], writes=[t_out])
            yield
            if ti in qset:
                yield from self._q_chain(u, ti, pb, W)
        self.run_il((tile(i, t) for i, t in enumerate(tiles)), depth=2)
        b.barrier()
        ar.release(m0)

    def phase_kv(self, tiles, setup):
        from types import SimpleNamespace as NS
        b, ar, l = self.b, self.ar, self.l
        m0 = ar.mark()
        mod, gm = setup
        wkv1 = ar.alloc([8, 160], BF16)
        wkv2 = ar.alloc([8, 256], BF16)
        wukv = ar.alloc([1024], BF16)
        t_w = Tok()
        wv = self.w_in[l].rearrange("(k p) c -> p k c", p=128)
        b.dma("pool", wkv1, wv[:, :, C_CKV:C_QB], writes=[t_w])
        b.dma("pool", wkv2, wv[:, :, C_KB:C_GA], writes=[t_w])
        b.dma("pool", wukv, self.w_ukv[l], writes=[t_w])
        gckv = ar.alloc([128], F32)
        gka = ar.alloc([96], F32)
        gkb = ar.alloc([64], F32)
        t_g = self.load_bcast(gckv, self.g_ckv[l:l + 1, :])
        t_g2 = self.load_bcast(gka, self.g_ka[l:l + 1, :])
        t_g3 = self.load_bcast(gkb, self.g_kb[l:l + 1, :])

        def mk():
            u = NS()
            for name, shp, dt_ in (("scr", [D], F32), ("st", [8], F32), ("h", [8, 128], BF16),
                                   ("hT", [8, 128], BF16), ("ckvn", [1, 128], BF16), ("ckvnT", [1, 128], BF16),
                                   ("ss8", [8], F32), ("ss2", [2], F32), ("sq", [8, 64], F32), ("kaf", [8, 64], F32),
                                   ("krf", [1, 32], F32), ("krr", [1, 32], F32), ("r1", [8, 64], F32),
                                   ("r2", [8, 64], F32), ("Kab", [8, 96], BF16), ("KaTs", [8, 128], BF16),
                                   ("Vab", [8, 66], BF16), ("kbf", [2, 64], F32), ("Kbb", [2, 64], BF16),
                                   ("KbTs", [2, 128], BF16), ("Vbb", [2, 66], BF16), ("sskr", [1], F32),
                                   ("junk", [128], F32)):
                setattr(u, name, ar.alloc(shp, dt_))
                setattr(u, "t_" + name, Tok())
            return u
        sets = [mk(), mk()]
        for u_ in sets:
            b.op("dve", lambda e, u_=u_: e.memset(u_.Vab[:, :, 64:66], 1.0), writes=[u_.t_Vab])
            b.op("dve", lambda e, u_=u_: e.memset(u_.Vbb[:, :, 64:66], 1.0), writes=[u_.t_Vbb])
        t_out = Tok()
        xts = [ar.alloc([D], F32) for _ in range(4)]
        t_xts = [Tok() for _ in range(4)]

        def issue(j):
            if j < len(tiles):
                tj = tiles[j]
                b.dma("sp", xts[j % 4], self.xsrc[tj * 128:(tj + 1) * 128, :], writes=[t_xts[j % 4]])
        issue(0)
        issue(1)

        def tile(idx, ti):
            u = sets[idx % 2]
            u.xt, u.t_xt = xts[idx % 4], t_xts[idx % 4]
            issue(idx + 2)
            pb = 4 * (idx % 2)
            PB_HT, PB_KV1, PB_KV2, PB_CT, PB_KVA, PB_KAT, PB_KBT = pb, pb + 1, pb + 2, pb, pb + 2, pb, pb + 1
            s = 1 if ti in self.ctx_tiles else 0
            yield from self.norm_mod_tile(u.xt, u.t_xt, gm[s], mod[(s, 0)], u.h.rearrange("p a b -> p (a b)"), u.t_h,
                                          u.scr, u.t_scr, u.st[:, 0:1], u.t_st)
            yield from self.transpose_rows(u.h, u.t_h, 8, 128, PB_HT, u.hT, u.t_hT)
            for kk in range(8):
                b.op("pe", lambda e, kk=kk: e.matmul(self.bank(PB_KV1)[:, 0:160], lhsT=u.hT[:, kk, :], rhs=wkv1[:, kk, :],
                                                     start=(kk == 0), stop=(kk == 7)),
                     reads=[u.t_hT, t_w], writes=[self.ptok[PB_KV1]])
            for kk in range(8):
                b.op("pe", lambda e, kk=kk: e.matmul(self.bank(PB_KV2)[:, 0:256], lhsT=u.hT[:, kk, :], rhs=wkv2[:, kk, :],
                                                     start=(kk == 0), stop=(kk == 7)),
                     reads=[u.t_hT, t_w], writes=[self.ptok[PB_KV2]])
            yield
            ckv = self.bank(PB_KV1)[:, 0:128]
            kr = self.bank(PB_KV1)[:, 128:160]
            b.op("act", lambda e: e.activation(out=u.junk, in_=ckv, func=AF.Square, accum_out=u.st[:, 1:2]),
                 reads=[self.ptok[PB_KV1]], writes=[u.t_junk, u.t_st])
            yield
            yield from self.rstd_from_ss(u.st[:, 1:2], 1, 128, u.t_st)
            b.op("dve", lambda e: e.scalar_tensor_tensor(out=u.ckvn[:, 0, :], in0=ckv, scalar=u.st[:, 1:2], in1=gckv,
                                                         op0=ALU.mult, op1=ALU.mult),
                 reads=[self.ptok[PB_KV1], u.t_st, t_g], writes=[u.t_ckvn])
            yield
            kbp = self.bank(PB_KV2)[:, 0:128].rearrange("p (h d) -> p h d", h=2, d=64)
            vbp = self.bank(PB_KV2)[:, 128:256].rearrange("p (h d) -> p h d", h=2, d=64)
            b.op("act", lambda e: e.activation(out=u.Vbb[:, :, 0:64], in_=vbp, func=AF.Copy), reads=[self.ptok[PB_KV2]],
                 writes=[u.t_Vbb])
            b.op("act", lambda e: e.activation(out=u.kbf, in_=kbp, func=AF.Copy), reads=[self.ptok[PB_KV2]],
                 writes=[u.t_kbf])
            yield
            b.dma("sp", self.Vb[:, ti, :], u.Vbb.rearrange("p a b -> p (a b)"), reads=[u.t_Vbb], writes=[t_out])
            b.op("act", lambda e: e.activation(out=u.junk[:, 0:32], in_=kr, func=AF.Square, accum_out=u.sskr),
                 reads=[self.ptok[PB_KV1]], writes=[u.t_junk, u.t_sskr])
            b.op("dve", lambda e: e.tensor_tensor(out=u.krf[:, 0, :], in0=kr, in1=gka[:, 64:96], op=ALU.mult),
                 reads=[self.ptok[PB_KV1], t_g2], writes=[u.t_krf])
            yield
            yield from self.transpose_rows(u.ckvn, u.t_ckvn, 1, 128, PB_CT, u.ckvnT, u.t_ckvnT, evac="dve")
            for j in range(2):
                b.op("pe", lambda e, j=j: e.matmul(self.bank(PB_KVA + j), lhsT=u.ckvnT[:, 0, :],
                                                   rhs=wukv[:, j * 512:(j + 1) * 512], start=True, stop=True),
                     reads=[u.t_ckvnT, t_w], writes=[self.ptok[PB_KVA + j]])
            yield
            kva = self.bank(PB_KVA, 2).rearrange("p (h d) -> p h d", h=8, d=128)
            t_kva = [self.ptok[PB_KVA], self.ptok[PB_KVA + 1]]
            knope = kva[:, :, 0:64]
            yield from self.rope_apply(u.krf, u.t_krf, 1, "a", ti, u.krr, u.t_krr, u.r1[:, 0:1, 0:32], u.r2[:, 0:1, 0:32],
                                       u.t_r1)
            b.op("act", lambda e: e.activation(out=u.kaf, in_=knope, func=AF.Copy), reads=t_kva, writes=[u.t_kaf])
            b.op("act", lambda e: e.activation(out=u.Vab[:, :, 0:64], in_=kva[:, :, 64:128], func=AF.Copy), reads=t_kva,
                 writes=[u.t_Vab])
            yield
            b.dma("sp", self.Va[:, ti, :], u.Vab.rearrange("p a b -> p (a b)"), reads=[u.t_Vab], writes=[t_out])
            b.op("act", lambda e: e.activation(out=u.sq, in_=u.kaf, func=AF.Square), reads=[u.t_kaf], writes=[u.t_sq])
            yield
            b.op("dve", lambda e: e.tensor_reduce(out=u.ss8, in_=u.sq, axis=AX.X, op=ALU.add), reads=[u.t_sq],
                 writes=[u.t_ss8])
            yield
            b.op("dve", lambda e: e.tensor_tensor(out=u.ss8, in0=u.ss8, in1=bc(u.sskr, [128, 8]), op=ALU.add),
                 reads=[u.t_ss8, u.t_sskr], writes=[u.t_ss8])
            yield
            yield from self.rstd_from_ss(u.ss8, 8, 96, u.t_ss8)
            b.op("dve", lambda e: e.tensor_tensor(out=u.kaf, in0=u.kaf, in1=bc(u.ss8.unsqueeze(2), [128, 8, 64]),
                                                  op=ALU.mult), reads=[u.t_kaf, u.t_ss8], writes=[u.t_kaf])
            yield
            b.op("dve", lambda e: e.tensor_tensor(out=u.Kab[:, :, 0:64], in0=u.kaf,
                                                  in1=bc(gka[:, 0:64].unsqueeze(1), [128, 8, 64]), op=ALU.mult),
                 reads=[u.t_kaf, t_g2], writes=[u.t_Kab])
            b.op("dve", lambda e: e.tensor_tensor(out=u.Kab[:, :, 64:96], in0=bc(u.krr, [128, 8, 32]),
                                                  in1=bc(u.ss8.unsqueeze(2), [128, 8, 32]), op=ALU.mult),
                 reads=[u.t_krr, u.t_ss8], writes=[u.t_Kab])
            yield
            yield from self.transpose_rows(u.Kab, u.t_Kab, 8, 96, PB_KAT, u.KaTs[0:96], u.t_KaTs)
            b.dma("sp", self.KaT[:, :, ti * 128:(ti + 1) * 128].rearrange("h d t -> d h t"), u.KaTs[0:96],
                  reads=[u.t_KaTs], writes=[t_out])
            yield from self.head_norm(u.kbf, u.t_kbf, 2, 64, None, gkb, t_g3, u.sq[:, 0:2, :], u.t_sq, u.ss2, u.t_ss2,
                                      u.kbf, u.t_kbf)
            yield from self.rope_apply(u.kbf, u.t_kbf, 2, "b", ti, u.Kbb, u.t_Kbb, u.r1[:, 0:2, :], u.r2[:, 0:2, :], u.t_r1)
            yield from self.transpose_rows(u.Kbb, u.t_Kbb, 2, 64, PB_KBT, u.KbTs[0:64], u.t_KbTs, evac="dve")
            b.dma("sp", self.KbT[:, :, ti * 128:(ti + 1) * 128].rearrange("h d t -> d h t"), u.KbTs[0:64],
                  reads=[u.t_KbTs], writes=[t_out])
            yield
        self.run_il((tile(i, t) for i, t in enumerate(tiles)), depth=2)
        b.barrier()
        ar.release(m0)

    def phase_q(self, tiles, setup):
        from types import SimpleNamespace as NS
        b, ar, l = self.b, self.ar, self.l
        m0 = ar.mark()
        mod, gm = setup
        wq1 = ar.alloc([8, 256], BF16)
        wq2 = ar.alloc([8, 512], BF16)
        wg = ar.alloc([8, 2048], BF16)
        wuq = ar.alloc([2, 768], BF16)
        t_w = Tok()
        wv = self.w_in[l].rearrange("(k p) c -> p k c", p=128)
        b.dma("pool", wq1, wv[:, :, C_CQ:C_CKV], writes=[t_w])
        b.dma("pool", wq2, wv[:, :, C_QB:C_KB], writes=[t_w])
        for j in range(4):
            b.dma("pool", wg[:, :, j * 512:(j + 1) * 512], wv[:, :, C_GA + j * 512:C_GA + (j + 1) * 512], writes=[t_w])
        b.dma("pool", wuq, self.w_uq[l].rearrange("(k p) c -> p k c", p=128), writes=[t_w])
        gcq = ar.alloc([256], F32)
        gqa = ar.alloc([96], F32)
        gqb = ar.alloc([64], F32)
        t_g = self.load_bcast(gcq, self.g_cq[l:l + 1, :])
        t_g2 = self.load_bcast(gqa, self.g_qa[l:l + 1, :])
        t_g3 = self.load_bcast(gqb, self.g_qb[l:l + 1, :])

        def mk():
            u = NS()
            for name, shp, dt_ in (("scr", [D], F32), ("st", [8], F32), ("h", [8, 128], BF16),
                                   ("hT", [8, 128], BF16), ("cqn", [2, 128], BF16), ("cqnT", [2, 128], BF16),
                                   ("ss8", [8], F32), ("ss8b", [8], F32), ("sq", [8, 96], F32), ("qaf", [8, 96], F32),
                                   ("qbf", [8, 64], F32), ("r1", [8, 64], F32), ("r2", [8, 64], F32),
                                   ("Qab", [8, 96], BF16), ("Qbb", [8, 64], BF16), ("QaTs", [8, 128], BF16),
                                   ("QbTs", [8, 128], BF16), ("Gsb", [2048], BF16), ("junk", [256], F32)):
                setattr(u, name, ar.alloc(shp, dt_))
                setattr(u, "t_" + name, Tok())
            return u
        sets = [mk(), mk()]
        t_out = Tok()
        xts = [ar.alloc([D], F32) for _ in range(4)]
        t_xts = [Tok() for _ in range(4)]

        def issue(j):
            if j < len(tiles):
                tj = tiles[j]
                b.dma("sp", xts[j % 4], self.xsrc[tj * 128:(tj + 1) * 128, :], writes=[t_xts[j % 4]])
        issue(0)
        issue(1)

        def tile(idx, ti):
            u = sets[idx % 2]
            u.xt, u.t_xt = xts[idx % 4], t_xts[idx % 4]
            issue(idx + 2)
            pb = 4 * (idx % 2)
            PB_HT, PB_CQ, PB_QB, PB_CT, PB_QA, PB_QTA, PB_QTB = pb, pb + 1, pb + 2, pb, pb + 2, pb + 2, pb + 3
            s = 1 if ti in self.ctx_tiles else 0
            yield from self.norm_mod_tile(u.xt, u.t_xt, gm[s], mod[(s, 0)], u.h.rearrange("p a b -> p (a b)"), u.t_h,
                                          u.scr, u.t_scr, u.st[:, 0:1], u.t_st)
            yield from self.transpose_rows(u.h, u.t_h, 8, 128, PB_HT, u.hT, u.t_hT)
            for kk in range(8):
                b.op("pe", lambda e, kk=kk: e.matmul(self.bank(PB_CQ)[:, 0:256], lhsT=u.hT[:, kk, :], rhs=wq1[:, kk, :],
                                                     start=(kk == 0), stop=(kk == 7)),
                     reads=[u.t_hT, t_w], writes=[self.ptok[PB_CQ]])
            for kk in range(8):
                b.op("pe", lambda e, kk=kk: e.matmul(self.bank(PB_QB), lhsT=u.hT[:, kk, :], rhs=wq2[:, kk, :],
                                                     start=(kk == 0), stop=(kk == 7)),
                     reads=[u.t_hT, t_w], writes=[self.ptok[PB_QB]])
            yield
            cq = self.bank(PB_CQ)[:, 0:256]
            b.op("act", lambda e: e.activation(out=u.junk, in_=cq, func=AF.Square, accum_out=u.st[:, 1:2]),
                 reads=[self.ptok[PB_CQ]], writes=[u.t_junk, u.t_st])
            qbp = self.bank(PB_QB).rearrange("p (h d) -> p h d", h=8, d=64)
            b.op("act", lambda e: e.activation(out=u.qbf, in_=qbp, func=AF.Copy), reads=[self.ptok[PB_QB]],
                 writes=[u.t_qbf])
            yield
            yield from self.rstd_from_ss(u.st[:, 1:2], 1, 256, u.t_st)
            b.op("dve", lambda e: e.scalar_tensor_tensor(out=u.cqn.rearrange("p a b -> p (a b)"), in0=cq,
                                                         scalar=u.st[:, 1:2], in1=gcq, op0=ALU.mult, op1=ALU.mult),
                 reads=[self.ptok[PB_CQ], u.t_st, t_g], writes=[u.t_cqn])
            yield
            yield from self.transpose_rows(u.cqn, u.t_cqn, 2, 128, PB_CT, u.cqnT, u.t_cqnT, evac="dve")
            for j, (c0, c1) in enumerate(((0, 512), (512, 768))):
                for kk in range(2):
                    b.op("pe", lambda e, j=j, kk=kk, c0=c0, c1=c1: e.matmul(
                        self.bank(PB_QA + j)[:, 0:c1 - c0], lhsT=u.cqnT[:, kk, :], rhs=wuq[:, kk, c0:c1],
                        start=(kk == 0), stop=(kk == 1)), reads=[u.t_cqnT, t_w], writes=[self.ptok[PB_QA + j]])
            for j in range(4):
                pbg = PB_CQ if j % 2 == 0 else PB_CT
                for kk in range(8):
                    b.op("pe", lambda e, kk=kk, j=j, pb=pbg: e.matmul(self.bank(pb), lhsT=u.hT[:, kk, :],
                                                                     rhs=wg[:, kk, j * 512:(j + 1) * 512],
                                                                     start=(kk == 0), stop=(kk == 7)),
                         reads=[u.t_hT, t_w], writes=[self.ptok[pbg]])
                b.op("act", lambda e, j=j, pb=pbg: e.activation(out=u.Gsb[:, j * 512:(j + 1) * 512], in_=self.bank(pb),
                                                                func=AF.Sigmoid), reads=[self.ptok[pbg]], writes=[u.t_Gsb])
                if j == 0:
                    qa = self.bank(PB_QA, 2)[:, 0:768].rearrange("p (h d) -> p h d", h=8, d=96)
                    b.op("act", lambda e: e.activation(out=u.qaf, in_=qa, func=AF.Copy),
                         reads=[self.ptok[PB_QA], self.ptok[PB_QA + 1]], writes=[u.t_qaf])
                yield
            b.dma("sp", self.Gs[ti * 128:(ti + 1) * 128, :], u.Gsb, reads=[u.t_Gsb], writes=[t_out])
            yield from self.head_norm(u.qaf, u.t_qaf, 8, 96, None, gqa, t_g2, u.sq, u.t_sq, u.ss8, u.t_ss8, u.qaf, u.t_qaf)
            b.op("act", lambda e: e.activation(out=u.Qab[:, :, 0:64], in_=u.qaf[:, :, 0:64], func=AF.Copy),
                 reads=[u.t_qaf], writes=[u.t_Qab])
            yield from self.rope_apply(u.qaf[:, :, 64:96], u.t_qaf, 8, "a", ti, u.Qab[:, :, 64:96], u.t_Qab,
                                       u.r1[:, :, 0:32], u.r2[:, :, 0:32], u.t_r1)
            yield from self.transpose_rows(u.Qab, u.t_Qab, 8, 96, PB_QTA, u.QaTs[0:96], u.t_QaTs)
            b.dma("sp", self.QaT[:, :, ti * 128:(ti + 1) * 128].rearrange("h d t -> d h t"), u.QaTs[0:96],
                  reads=[u.t_QaTs], writes=[t_out])
            yield from self.head_norm(u.qbf, u.t_qbf, 8, 64, None, gqb, t_g3, u.sq[:, :, 0:64], u.t_sq, u.ss8b, u.t_ss8b,
                                      u.qbf, u.t_qbf)
            yield from self.rope_apply(u.qbf, u.t_qbf, 8, "b", ti, u.Qbb, u.t_Qbb, u.r1, u.r2, u.t_r1)
            yield from self.transpose_rows(u.Qbb, u.t_Qbb, 8, 64, PB_QTB, u.QbTs[0:64], u.t_QbTs, evac="dve")
            b.dma("sp", self.QbT[:, :, ti * 128:(ti + 1) * 128].rearrange("h d t -> d h t"), u.QbTs[0:64],
                  reads=[u.t_QbTs], writes=[t_out])
            yield
        self.run_il((tile(i, t) for i, t in enumerate(tiles)), depth=2)
        b.barrier()
        ar.release(m0)

    def phase_attn(self, blocks):
        b, ar, l = self.b, self.ar, self.l
        m0 = ar.mark()
        Vas = ar.alloc([NKT, 8, 66], BF16)
        Vbs = ar.alloc([NKT, 2, 66], BF16)
        t_V = Tok()
        for k0 in range(0, NKT, 9):
            k1 = min(NKT, k0 + 9)
            b.dma("sp", Vas[:, k0:k1].rearrange("p k h d -> p k (h d)"), self.Va[:, k0:k1, :], writes=[t_V])
        b.dma("sp", Vbs.rearrange("p k h d -> p k (h d)"), self.Vb, writes=[t_V])
        KT = [ar.alloc([NKEY], BF16) for _ in range(2)]
        t_KT = [Tok(), Tok()]
        QT = [ar.alloc([self.NTOK], BF16) for _ in range(2)]
        t_QT = [Tok(), Tok()]
        for i_ in range(2):
            b.op("dve", lambda e, i_=i_: e.memset(KT[i_][64:128], 0.0), writes=[t_KT[i_]])
            b.op("dve", lambda e, i_=i_: e.memset(QT[i_][64:128], 0.0), writes=[t_QT[i_]])
        PT = [ar.alloc([1024], BF16) for _ in range(3)]
        t_PT = [Tok() for _ in range(3)]
        OTs = [ar.alloc([512], BF16) for _ in range(2)]
        t_OTs = [Tok(), Tok()]
        rd = [ar.alloc([512], F32) for _ in range(2)]
        t_rd = [Tok(), Tok()]
        otf = [ar.alloc([512], F32) for _ in range(2)]
        t_otf = [Tok(), Tok()]
        onesb = ar.alloc([64], BF16)
        t_onesb = Tok()
        b.op("dve", lambda e: e.memset(onesb, 1.0), writes=[t_onesb])
        rdh = [ar.alloc([512], BF16) for _ in range(2)]
        rdl = [ar.alloc([512], BF16) for _ in range(2)]
        PS_S, PS_O, PS_BC = (0, 1, 2, 3), (4, 5), 6
        t_out = Tok()
        from types import SimpleNamespace as NS

        def _ldh(hj):
            if hj < 8:
                dj, ks, qs = 96, self.KaT[hj], self.QaT[hj]
            else:
                dj, ks, qs = 64, self.KbT[(hj - 8) // 4], self.QbT[hj - 8]
            if hj in (8, 9):
                b.op("dve", lambda e: e.memset(KT[hj % 2][64:128], 0.0), writes=[t_KT[hj % 2]])
                b.op("dve", lambda e: e.memset(QT[hj % 2][64:128], 0.0), writes=[t_QT[hj % 2]])
            b.dma("sp", KT[hj % 2][0:dj], ks, writes=[t_KT[hj % 2]])
            b.dma("sp", QT[hj % 2][0:dj], qs, writes=[t_QT[hj % 2]])

        items = []
        octr = 0
        for hh in range(16):
            if hh < 8:
                scale = SCALE_A
                vsl = lambda kt, hh=hh: Vas[:, kt, hh, 0:65]
            else:
                scale = SCALE_B
                vsl = lambda kt, g=hh - 8: Vbs[:, kt, g // 4, 0:65]
            for bi, (q0, nq, kts) in enumerate(blocks):
                groups = [kts[i:i + 2] for i in range(0, len(kts), 2)]
                for gi, grp in enumerate(groups):
                    items.append(NS(hh=hh, hb=hh % 2, q0=q0, nq=nq, ob=PS_O[octr % 2], op_=octr % 2, gi=gi, grp=grp,
                                    nk=len(kts), first_of_head=(bi == 0 and gi == 0), last=(gi == len(groups) - 1),
                                    vsl=vsl, scale=scale, idx=len(items)))
                octr += 1
        n = len(items)
        deferred = []

        def emit_S(it):
            sp_ = it.idx % 2
            pt = it.idx % 3
            it.pt = pt
            sbanks = [PS_S[2 * sp_ + j] for j in range(len(it.grp))]
            for j, kt in enumerate(it.grp):
                b.op("pe", lambda e, sb=sbanks[j], kt=kt: e.matmul(
                    self.bank(sb)[:, 0:it.nq], lhsT=KT[it.hb][:, kt * 128:(kt + 1) * 128],
                    rhs=QT[it.hb][:, it.q0:it.q0 + it.nq], start=True, stop=True),
                    reads=[t_KT[it.hb], t_QT[it.hb]], writes=[self.ptok[sbanks[j]]])
            if it.nq == 512 and len(it.grp) == 2:
                b.op("act", lambda e: e.activation(out=PT[pt], in_=self.bank(sbanks[0], 2), func=AF.Exp, scale=it.scale),
                     reads=[self.ptok[x] for x in sbanks], writes=[t_PT[pt]])
            else:
                for j in range(len(it.grp)):
                    b.op("act", lambda e, j=j: e.activation(
                        out=PT[pt][:, j * 512:j * 512 + it.nq], in_=self.bank(sbanks[j])[:, 0:it.nq], func=AF.Exp,
                        scale=it.scale), reads=[self.ptok[sbanks[j]]], writes=[t_PT[pt]])

        def emit_PV(it):
            for j, pkt in enumerate(it.grp):
                pi = 2 * it.gi + j
                b.op("pe", lambda e, j=j, pkt=pkt, pi=pi: e.matmul(
                    self.bank(it.ob)[0:65, 0:it.nq], lhsT=it.vsl(pkt), rhs=PT[it.pt][:, j * 512:j * 512 + it.nq],
                    start=(pi == 0), stop=(pi == it.nk - 1)), reads=[t_V, t_PT[it.pt]], writes=[self.ptok[it.ob]])

        def emit_norm_head(it):
            ob, nq, op_ = it.ob, it.nq, it.op_
            b.op("dve", lambda e: e.reciprocal(out=rd[op_][64:65, 0:nq], in_=self.bank(ob)[64:65, 0:nq]),
                 reads=[self.ptok[ob]], writes=[t_rd[op_]])
            b.op("dve", lambda e: e.tensor_copy(out=otf[op_][0:64, 0:nq], in_=self.bank(ob)[0:64, 0:nq]),
                 reads=[self.ptok[ob]], writes=[t_otf[op_]])
            b.op("dve", lambda e: e.tensor_copy(out=rdh[op_][64:65, 0:nq], in_=rd[op_][64:65, 0:nq]),
                 reads=[t_rd[op_]], writes=[t_rd[op_]])
            b.op("dve", lambda e: e.tensor_tensor(out=rdl[op_][64:65, 0:nq], in0=rd[op_][64:65, 0:nq],
                                                  in1=rdh[op_][64:65, 0:nq], op=ALU.subtract),
                 reads=[t_rd[op_]], writes=[t_rd[op_]])

            def tail():
                b.op("pe", lambda e: e.matmul(self.bank(PS_BC)[0:64, 0:nq], lhsT=onesb[64:65, 0:64],
                                              rhs=rdh[op_][64:65, 0:nq], start=True, stop=False),
                     reads=[t_rd[op_], t_onesb], writes=[self.ptok[PS_BC]])
                b.op("pe", lambda e: e.matmul(self.bank(PS_BC)[0:64, 0:nq], lhsT=onesb[64:65, 0:64],
                                              rhs=rdl[op_][64:65, 0:nq], start=False, stop=True),
                     reads=[t_rd[op_], t_onesb], writes=[self.ptok[PS_BC]])
                b.op("dve", lambda e: e.tensor_tensor(
                    out=OTs[op_][0:64, 0:nq], in0=otf[op_][0:64, 0:nq], in1=self.bank(PS_BC)[0:64, 0:nq],
                    op=ALU.mult), reads=[t_otf[op_], self.ptok[PS_BC]], writes=[t_OTs[op_]])
                b.dma("sp", self.OT[it.hh, :, it.q0:it.q0 + nq], OTs[op_][0:64, 0:nq], reads=[t_OTs[op_]],
                      writes=[t_out])
            return tail

        for t in range(n + 1):
            if t < n:
                it = items[t]
                if it.first_of_head:
                    if it.hh == 0:
                        _ldh(0)
                    if it.hh + 1 < 16:
                        _ldh(it.hh + 1)
                emit_S(it)
            for due, fn in [x for x in deferred if x[0] <= t]:
                fn()
            deferred = [x for x in deferred if x[0] > t]
            if t >= 1:
                pit = items[t - 1]
                emit_PV(pit)
                if pit.last:
                    deferred.append((t + 6, emit_norm_head(pit)))
        for due, fn in deferred:
            fn()
        b.barrier()
        ar.release(m0)

    def phase_merge(self, tiles):
        from types import SimpleNamespace as NS
        b, ar, l = self.b, self.ar, self.l
        m0 = ar.mark()
        mod = self.emit_mod([2])
        woa = ar.alloc([4, 1024], BF16)
        wob = ar.alloc([4, 1024], BF16)
        wout = ar.alloc([8, 1024], BF16)
        t_w = Tok()
        b.dma("pool", woa, self.w_oa[l].rearrange("(k p) c -> p k c", p=128), writes=[t_w])
        b.dma("pool", wob, self.w_ob[l].rearrange("(k p) c -> p k c", p=128), writes=[t_w])
        b.dma("pool", wout, self.w_out[l].rearrange("(k p) c -> p k c", p=128), writes=[t_w])

        def mk():
            u = NS()
            for name, shp, dt_ in (("t1", [D], F32), ("t2", [D], F32), ("y", [8, 128], BF16), ("yT", [8, 128], BF16)):
                setattr(u, name, ar.alloc(shp, dt_))
                setattr(u, "t_" + name, Tok())
            return u
        sets = [mk(), mk()]
        OT2 = self.OT.rearrange("h d t -> (h d) t")
        lds = []
        for _ in range(4):
            lds.append(NS(OTt=ar.alloc([8, 128], BF16), Gt=ar.alloc([2048], BF16), xt=ar.alloc([D], F32),
                          t_OTt=Tok(), t_Gt=Tok(), t_xt=Tok()))

        def issue(j):
            if j < len(tiles):
                tj, v = tiles[j], lds[j % 4]
                b.dma("sp", v.OTt, OT2[:, tj * 128:(tj + 1) * 128].rearrange("(k p) t -> p k t", p=128), writes=[v.t_OTt])
                b.dma("sp", v.Gt, self.Gs[tj * 128:(tj + 1) * 128, :], writes=[v.t_Gt])
                b.dma("sp", v.xt, self.xsrc[tj * 128:(tj + 1) * 128, :], writes=[v.t_xt])
        issue(0)
        issue(1)

        def tile(idx, ti):
            u = sets[idx % 2]
            v = lds[idx % 4]
            u.OTt, u.Gt, u.xt, u.t_OTt, u.t_Gt, u.t_xt = v.OTt, v.Gt, v.xt, v.t_OTt, v.t_Gt, v.t_xt
            s = 1 if ti in self.ctx_tiles else 0
            issue(idx + 2)
            pbase = 4 * (idx % 2)
            for br, w_ in ((0, woa), (1, wob)):
                for j in range(2):
                    pb = pbase + 2 * br + j
                    for kk in range(4):
                        b.op("pe", lambda e, pb=pb, kk=kk, br=br, w_=w_, j=j: e.matmul(
                            self.bank(pb), lhsT=u.OTt[:, 4 * br + kk, :], rhs=w_[:, kk, j * 512:(j + 1) * 512],
                            start=(kk == 0), stop=(kk == 3)), reads=[u.t_OTt, t_w], writes=[self.ptok[pb]])
            yield
            b.op("dve", lambda e: e.tensor_tensor(out=u.t1, in0=self.bank(pbase, 2), in1=u.Gt[:, 0:1024], op=ALU.mult),
                 reads=[self.ptok[pbase], self.ptok[pbase + 1], u.t_Gt], writes=[u.t_t1])
            yield
            b.op("dve", lambda e: e.tensor_tensor(out=u.t2, in0=self.bank(pbase + 2, 2), in1=u.Gt[:, 1024:2048],
                                                  op=ALU.mult),
                 reads=[self.ptok[pbase + 2], self.ptok[pbase + 3], u.t_Gt], writes=[u.t_t2])
            yield
            b.op("dve", lambda e: e.tensor_tensor(out=u.y.rearrange("p a b -> p (a b)"), in0=u.t1, in1=u.t2, op=ALU.add),
                 reads=[u.t_t1, u.t_t2], writes=[u.t_y])
            yield
            yield from self.transpose_rows(u.y, u.t_y, 8, 128, pbase, u.yT, u.t_yT)
            for j in range(2):
                for kk in range(8):
                    b.op("pe", lambda e, j=j, kk=kk: e.matmul(self.bank(pbase + 1 + j), lhsT=u.yT[:, kk, :],
                                                              rhs=wout[:, kk, j * 512:(j + 1) * 512],
                                                              start=(kk == 0), stop=(kk == 7)),
                         reads=[u.t_yT, t_w], writes=[self.ptok[pbase + 1 + j]])
            yield
            gt1, t_gt1 = mod[(s, 2)]
            b.op("dve", lambda e: e.tensor_tensor(out=u.t1, in0=self.bank(pbase + 1, 2), in1=gt1, op=ALU.mult),
                 reads=[self.ptok[pbase + 1], self.ptok[pbase + 2], t_gt1], writes=[u.t_t1])
            yield
            b.op("dve", lambda e: e.tensor_tensor(out=self.acc[:, idx, :], in0=u.t1, in1=u.xt, op=ALU.add),
                 reads=[u.t_t1, u.t_xt], writes=[self.t_acc[idx]])
            yield
        self.run_il((tile(i, t) for i, t in enumerate(tiles)), depth=2)
        b.barrier()
        ar.release(m0)

    def phase_moe(self, tiles, dst):
        from types import SimpleNamespace as NS
        b, ar, l = self.b, self.ar, self.l
        NT = 17
        ntile = len(tiles)
        isctx = [t in self.ctx_tiles for t in tiles]
        has_ctx = any(isctx)
        m0 = ar.mark()
        fT = ar.alloc([8, NT * 128], BF16)
        t_fT = [Tok() for _ in range(NT)]
        gate = ar.alloc([NT, 16], F32)
        t_gate = [Tok() for _ in range(NT)]
        modg = self.emit_mod([5])
        m1 = ar.mark()
        mod = self.emit_mod([3, 4])
        gff = ar.alloc([D], F32)
        t_gff = self.load_bcast(gff, self.g_ffn[l:l + 1, :])
        gm = {}
        for s in (0, 1):
            ap, tk = mod[(s, 4)]
            b.op("dve", lambda e, ap=ap: e.scalar_tensor_tensor(out=ap, in0=ap, scalar=1.0, in1=gff, op0=ALU.add,
                                                                op1=ALU.mult), reads=[tk, t_gff], writes=[tk])
            gm[s] = (ap, tk)
        wr = ar.alloc([8, 16], F32)
        t_wr = Tok()
        b.dma("sp", wr, self.w_router.rearrange("(k p) e -> p k e", p=128), writes=[t_wr])
        brt = ar.alloc([16], F32)
        t_brt = self.load_bcast(brt, self.b_router)

        def mk():
            u = NS()
            for name, shp, dt_ in (("ff", [8, 128], F32), ("scr", [D], F32), ("st", [8], F32), ("fT32", [8, 128], F32),
                                   ("sc", [4, 4], F32), ("sel", [4, 4], F32), ("eq", [4, 4], F32), ("g2", [4, 4], F32),
                                   ("m1t", [4], F32), ("m2t", [4], F32), ("gs", [4], F32), ("gmx", [2], F32),
                                   ("gmask", [4], F32)):
                setattr(u, name, ar.alloc(shp, dt_))
                setattr(u, "t_" + name, Tok())
            return u
        sets = [mk(), mk()]

        def ptile(ti):
            u = sets[ti % 2]
            s = 1 if isctx[ti] else 0
            pb0 = 4 * (ti % 2)
            yield from self.norm_mod_tile(self.acc[:, ti, :], self.t_acc[ti], gm[s], mod[(s, 3)],
                                          u.ff.rearrange("p a b -> p (a b)"), u.t_ff, u.scr, u.t_scr, u.st[:, 0:1], u.t_st)
            for kk in range(8):
                b.op("pe", lambda e, kk=kk: e.transpose(self.bank(pb0, 2)[:, kk * 128:(kk + 1) * 128], u.ff[:, kk, :],
                                                        self.identf), reads=[u.t_ff, self.t_identf],
                     writes=[self.ptok[pb0], self.ptok[pb0 + 1]])
            yield
            src = self.bank(pb0, 2).rearrange("p (a b) -> p a b", a=8, b=128)
            b.op("act", lambda e: e.activation(out=u.fT32, in_=src, func=AF.Copy),
                 reads=[self.ptok[pb0], self.ptok[pb0 + 1]], writes=[u.t_fT32])
            b.op("dve", lambda e: e.tensor_copy(out=fT[:, :, ti * 128:(ti + 1) * 128], in_=src),
                 reads=[self.ptok[pb0], self.ptok[pb0 + 1]], writes=[t_fT[ti]])
            yield
            for kk in range(8):
                b.op("pe", lambda e, kk=kk: e.matmul(self.bank(pb0 + 2)[:, 0:16], lhsT=u.fT32[:, kk, :], rhs=wr[:, kk, :],
                                                     start=(kk == 0), stop=(kk == 7)), reads=[u.t_fT32, t_wr],
                     writes=[self.ptok[pb0 + 2]])
            yield
            scf = u.sc.rearrange("p a b -> p (a b)")
            g2f = u.g2.rearrange("p a b -> p (a b)")
            steps = [
                ("act", lambda e: e.activation(out=scf, in_=self.bank(pb0 + 2)[:, 0:16], func=AF.Sigmoid),
                 [self.ptok[pb0 + 2]], [u.t_sc]),
                ("dve", lambda e: e.tensor_tensor(out=u.sel.rearrange("p a b -> p (a b)"), in0=scf, in1=brt, op=ALU.add),
                 [u.t_sc, t_brt], [u.t_sel]),
                ("dve", lambda e: e.tensor_reduce(out=u.m1t, in_=u.sel, axis=AX.X, op=ALU.max), [u.t_sel], [u.t_m1t]),
                ("dve", lambda e: e.tensor_tensor(out=u.eq, in0=u.sel, in1=bc(u.m1t.unsqueeze(2), [128, 4, 4]),
                                                  op=ALU.is_equal), [u.t_sel, u.t_m1t], [u.t_eq]),
                ("dve", lambda e: e.scalar_tensor_tensor(out=u.g2, in0=u.eq, scalar=-1e30, in1=u.sel, op0=ALU.mult,
                                                         op1=ALU.add), [u.t_eq, u.t_sel], [u.t_g2]),
                ("dve", lambda e: e.tensor_reduce(out=u.m2t, in_=u.g2, axis=AX.X, op=ALU.max), [u.t_g2], [u.t_m2t]),
                ("dve", lambda e: e.tensor_tensor(out=u.gs, in0=u.m1t, in1=u.m2t, op=ALU.add), [u.t_m1t, u.t_m2t],
                 [u.t_gs]),
                ("dve", lambda e: e.tensor_reduce(out=u.gmx[:, 0:1], in_=u.gs, axis=AX.X, op=ALU.max), [u.t_gs],
                 [u.t_gmx]),
                ("dve", lambda e: e.tensor_scalar(out=u.gmask, in0=u.gs, scalar1=u.gmx[:, 0:1], scalar2=None,
                                                  op0=ALU.is_equal), [u.t_gs, u.t_gmx], [u.t_gmask]),
                ("dve", lambda e: e.tensor_tensor(out=u.eq, in0=u.sel, in1=bc(u.m2t.unsqueeze(2), [128, 4, 4]),
                                                  op=ALU.is_ge), [u.t_sel, u.t_m2t], [u.t_eq]),
                ("dve", lambda e: e.tensor_tensor(out=u.eq, in0=u.eq, in1=bc(u.gmask.unsqueeze(2), [128, 4, 4]),
                                                  op=ALU.mult), [u.t_eq, u.t_gmask], [u.t_eq]),
                ("dve", lambda e: e.tensor_tensor(out=u.g2, in0=u.eq, in1=u.sc, op=ALU.mult), [u.t_eq, u.t_sc], [u.t_g2]),
                ("dve", lambda e: e.tensor_reduce(out=u.gmx[:, 1:2], in_=g2f, axis=AX.X, op=ALU.add), [u.t_g2],
                 [u.t_gmx]),
                ("dve", lambda e: e.reciprocal(out=u.gmx[:, 1:2], in_=u.gmx[:, 1:2]), [u.t_gmx], [u.t_gmx]),
                ("dve", lambda e: e.tensor_scalar(out=gate[:, ti, :], in0=g2f, scalar1=u.gmx[:, 1:2], scalar2=None,
                                                  op0=ALU.mult), [u.t_g2, u.t_gmx], [t_gate[ti]]),
            ]
            for eng_, fn_, rd_, wr_ in steps:
                b.op(eng_, fn_, reads=rd_, writes=wr_)
                yield
        self.run_il((ptile(t) for t in range(ntile)), depth=2)
        b.barrier()
        ar.release(m1)
        wgs = [ar.alloc([8, 512], BF16) for _ in range(2)]
        wus = [ar.alloc([8, 512], BF16) for _ in range(2)]
        t_wgu = [Tok(), Tok()]
        wd32 = ar.alloc([4, 1024], F32)
        t_wd32 = Tok()
        wdl = [ar.alloc([4, 1024], BF16) for _ in range(2)]
        wdc = [ar.alloc([4, 1024], BF16) for _ in range(2)]
        t_wd = [Tok(), Tok()]
        sg = [ar.alloc([512], F32) for _ in range(2)]
        t_sg = [Tok(), Tok()]
        AT = [ar.alloc([4, 512], BF16) for _ in range(2)]
        t_AT = [Tok(), Tok()]
        blocks = [(q * 512, min(512, ntile * 128 - q * 512)) for q in range((ntile + 3) // 4)]
        gctr = 0
        yctr = 0
        actr = 0
        for e_ in range(17):
            wb = e_ % 2
            if e_ == 0:
                sg_, su_, sd_ = self.w_s_gate[l], self.w_s_up[l], self.w_s_down[l]
            else:
                sg_, su_, sd_ = self.w_e_gate[l, e_ - 1], self.w_e_up[l, e_ - 1], self.w_e_down[l, e_ - 1]
            def _ldw(ej, only_d=False, only_gu=False):
                if ej == 0:
                    a_, b_, c_ = self.w_s_gate[l], self.w_s_up[l], self.w_s_down[l]
                else:
                    a_, b_, c_ = self.w_e_gate[l, ej - 1], self.w_e_up[l, ej - 1], self.w_e_down[l, ej - 1]
                if not only_d:
                    b.dma("pool", wgs[ej % 2], a_.rearrange("(k p) f -> p k f", p=128), writes=[t_wgu[ej % 2]])
                    b.dma("pool", wus[ej % 2], b_.rearrange("(k p) f -> p k f", p=128), writes=[t_wgu[ej % 2]])
                if not only_gu:
                    b.dma("sp", wd32, c_.rearrange("(k p) c -> p k c", p=128), writes=[t_wd32])
            if e_ == 0:
                _ldw(0)
            if e_ + 1 < 17:
                _ldw(e_ + 1, only_gu=True)
            b.op("dve", lambda e, wb=wb: e.tensor_tensor(out=wdl[wb], in0=wd32,
                                                         in1=bc(modg[(0, 5)][0].unsqueeze(1), [128, 4, 1024]), op=ALU.mult),
                 reads=[t_wd32, modg[(0, 5)][1]], writes=[t_wd[wb]])
            if has_ctx:
                b.op("dve", lambda e, wb=wb: e.tensor_tensor(out=wdc[wb], in0=wd32,
                                                             in1=bc(modg[(1, 5)][0].unsqueeze(1), [128, 4, 1024]),
                                                             op=ALU.mult),
                     reads=[t_wd32, modg[(1, 5)][1]], writes=[t_wd[wb]])
            if e_ + 1 < 17:
                _ldw(e_ + 1, only_d=True)
            for (t0, nt) in blocks:
                ab = actr % 2
                actr += 1
                tiles = list(range(t0 // 128, (t0 + nt) // 128))
                for fc in range(4):
                    gb = gctr % 2
                    gctr += 1
                    for kk in range(8):
                        b.op("pe", lambda e, gb=gb, kk=kk, fc=fc, wb=wb, t0=t0, nt=nt: e.matmul(
                            self.bank(gb)[:, 0:nt], lhsT=wgs[wb][:, kk, fc * 128:(fc + 1) * 128], rhs=fT[:, kk, t0:t0 + nt],
                            start=(kk == 0), stop=(kk == 7)), reads=[t_wgu[wb]] + [t_fT[t] for t in tiles],
                            writes=[self.ptok[gb]])
                    for kk in range(8):
                        b.op("pe", lambda e, gb=gb, kk=kk, fc=fc, wb=wb, t0=t0, nt=nt: e.matmul(
                            self.bank(2 + gb)[:, 0:nt], lhsT=wus[wb][:, kk, fc * 128:(fc + 1) * 128],
                            rhs=fT[:, kk, t0:t0 + nt], start=(kk == 0), stop=(kk == 7)),
                            reads=[t_wgu[wb]] + [t_fT[t] for t in tiles], writes=[self.ptok[2 + gb]])
                    b.op("act", lambda e, gb=gb, nt=nt: e.activation(out=sg[gb][:, 0:nt], in_=self.bank(gb)[:, 0:nt],
                                                                      func=AF.Silu), reads=[self.ptok[gb]], writes=[t_sg[gb]])
                    b.op("dve", lambda e, gb=gb, nt=nt, ab=ab, fc=fc: e.tensor_tensor(
                        out=AT[ab][:, fc, 0:nt], in0=sg[gb][:, 0:nt], in1=self.bank(2 + gb)[:, 0:nt], op=ALU.mult),
                        reads=[t_sg[gb], self.ptok[2 + gb]], writes=[t_AT[ab]])
                for i, ti in enumerate(tiles):
                    yb = 4 + 2 * (yctr % 2)
                    yctr += 1
                    wd_ = wdc[wb] if isctx[ti] else wdl[wb]
                    for j in range(2):
                        for fc in range(4):
                            b.op("pe", lambda e, yb=yb, j=j, fc=fc, ab=ab, i=i, wd_=wd_: e.matmul(
                                self.bank(yb + j), lhsT=AT[ab][:, fc, i * 128:(i + 1) * 128],
                                rhs=wd_[:, fc, j * 512:(j + 1) * 512], start=(fc == 0), stop=(fc == 3)),
                                reads=[t_AT[ab], t_wd[wb]], writes=[self.ptok[yb + j]])
                    if e_ == 0:
                        b.op("dve", lambda e, yb=yb, ti=ti: e.tensor_tensor(out=self.acc[:, ti, :], in0=self.bank(yb, 2),
                                                                            in1=self.acc[:, ti, :], op=ALU.add),
                             reads=[self.ptok[yb], self.ptok[yb + 1]], writes=[self.t_acc[ti]])
                    else:
                        b.op("dve", lambda e, yb=yb, ti=ti, e_=e_: e.scalar_tensor_tensor(
                            out=self.acc[:, ti, :], in0=self.bank(yb, 2), scalar=gate[:, ti, e_ - 1:e_],
                            in1=self.acc[:, ti, :], op0=ALU.mult, op1=ALU.add),
                            reads=[self.ptok[yb], self.ptok[yb + 1], t_gate[ti]], writes=[self.t_acc[ti]])
        t_out = Tok()
        for ti in range(ntile):
            b.dma("sp", dst(ti), self.acc[:, ti, :], reads=[self.t_acc[ti]], writes=[t_out])
        b.barrier()
        ar.release(m0)

    def build(self):
        self.setup_consts()
        allk = list(range(NKT))
        if self.mode == "A":
            self.phase_kv(list(range(NT)))
        elif self.mode == "B":
            tiles = list(range(NLAT_T if self.last else NT))
            blocks = [(q * 512, 512, allk) for q in range(4)]
            if not self.last:
                blocks.append((2048, 128, self.ctx_keys))
            self.phase_q(tiles)
            self.phase_attn(blocks)
            self.acc = self.ar.alloc([NT, D], F32)
            self.t_acc = [Tok() for _ in range(NT)]
            self.phase_merge(tiles)
            self.phase_moe(tiles, lambda i: self.xout[i * 128:(i + 1) * 128, :])
        else:
            self.l, self.last, self.xsrc = 0, False, self.xin
            alltiles = list(range(34))
            msetup = self.ar.mark()
            setup = self._common_setup()
            self.phase_kvq(alltiles, alltiles, setup)
            self.ar.release(msetup)
            blocks = [(q * 512, 512, allk) for q in range(8)] + [(4096, 256, self.ctx_keys)]
            self.phase_attn(blocks)
            macc = self.ar.mark()
            self.acc = self.ar.alloc([NT, D], F32)
            self.t_acc = [Tok() for _ in range(NT)]
            for tl in (alltiles[0:17], alltiles[17:34]):
                self.phase_merge(tl)
                self.phase_moe(tl, lambda i, tl=tl: self.xs1[tl[i] * 128:(tl[i] + 1) * 128, :])
            self.b.barrier()
            self.ar.release(macc)
            self.l, self.last, self.xsrc = 1, True, self.xs1
            own = list(range(16))
            msetup = self.ar.mark()
            setup = self._common_setup()
            self.phase_kvq(alltiles, own, setup)
            self.ar.release(msetup)
            self.phase_attn([(q * 512, 512, allk) for q in range(4)])
            self.acc = self.ar.alloc([NT, D], F32)
            self.t_acc = [Tok() for _ in range(NT)]
            self.phase_merge(own)
            self.phase_moe(own, lambda i: self.xout[i * 128:(i + 1) * 128, :])
        self.b.barrier()
        self.es.close()
        return self.nc


def _common_inputs(inputs, core):
    bidx, hf = core // 2, core % 2
    x = np.asarray(inputs["x"], dtype=np.float32)
    ctx = np.asarray(inputs["ctx"], dtype=np.float32)
    xin = np.concatenate([x[bidx, hf * 2048:(hf + 1) * 2048], ctx[bidx, hf * 128:(hf + 1) * 128]], axis=0)
    cvec = np.stack([np.asarray(inputs["c"], np.float32)[bidx], np.asarray(inputs["c_ctx"], np.float32)], 0)
    cvecT = np.ascontiguousarray(cvec.reshape(2, 8, 128).transpose(2, 0, 1))
    n = hf * 2048 + np.arange(2048)
    rowpos = np.zeros((128, NT), np.float32)
    colpos = np.zeros((128, NT), np.float32)
    rowpos[:, :16] = (n // 64).reshape(16, 128).T
    colpos[:, :16] = (n % 64).reshape(16, 128).T
    return dict(xin=np.ascontiguousarray(xin), cvecT=cvecT, ident=np.eye(128, dtype=np.float32), rowpos=rowpos,
                colpos=colpos)


_PROGS = {}


def _prog(mode, layer):
    k = (mode, layer)
    if k not in _PROGS:
        _PROGS[k] = Prog(mode, layer).build()
    return _PROGS[k]


def _f(inputs, k, l=None):
    a = np.asarray(inputs[k], np.float32)
    if l is not None:
        a = a[l:l + 1]
    return np.ascontiguousarray(a)


def run_A(inputs, layer, xins):
    nc = _prog("A", layer)
    shared = {k: _f(inputs, k, layer) for k in ("g_attn", "w_in", "g_ckv", "w_ukv", "g_ka", "g_kb")}
    shared["w_mod"] = np.ascontiguousarray(np.asarray(inputs["w_mod"], np.float32)[layer:layer + 1, :, :2 * D])
    shared["b_mod"] = np.ascontiguousarray(np.asarray(inputs["b_mod"], np.float32)[layer:layer + 1, :2 * D])
    in_maps = []
    for c in range(NCORES):
        d = _common_inputs(inputs, c)
        d["xin"] = xins[c]
        d.update(shared)
        in_maps.append(d)
    res = run_bass_kernel_spmd(nc, in_maps, core_ids=list(range(NCORES)))
    return res.results


def assemble_kv(resA):
    out = []
    for bidx in range(NB):
        r0, r1 = resA[2 * bidx], resA[2 * bidx + 1]
        d = {}
        for k in ("KaT", "KbT"):
            a0, a1 = np.asarray(r0[k]), np.asarray(r1[k])
            d[k] = np.ascontiguousarray(np.concatenate([a0[..., 2048:], a1[..., 2048:], a0[..., :2048], a1[..., :2048]], -1))
        for k in ("Va", "Vb"):
            a0, a1 = np.asarray(r0[k]), np.asarray(r1[k])
            d[k] = np.ascontiguousarray(np.concatenate([a0[2048:], a1[2048:], a0[:2048], a1[:2048]], 0))
        out.append(d)
    return out


def run_B(inputs, layer, xins, kv):
    nc = _prog("B", layer)
    names = ("w_mod", "b_mod", "g_attn", "w_in", "g_ffn", "g_cq", "w_uq", "g_qa", "g_qb", "w_oa", "w_ob", "w_out",
             "w_e_gate", "w_e_up", "w_e_down", "w_s_gate", "w_s_up", "w_s_down")
    shared = {k: _f(inputs, k, layer) for k in names}
    shared["w_router"] = _f(inputs, "w_router")
    shared["b_router"] = _f(inputs, "b_router").reshape(1, 16)
    in_maps = []
    for c in range(NCORES):
        d = _common_inputs(inputs, c)
        d["xin"] = xins[c]
        d.update(shared)
        d.update(kv[c // 2])
        in_maps.append(d)
    res = run_bass_kernel_spmd(nc, in_maps, core_ids=list(range(NCORES)))
    return res.results


def _fused_inputs(inputs, core):
    bidx, hf = core // 2, core % 2
    x = np.asarray(inputs["x"], dtype=np.float32)
    ctx = np.asarray(inputs["ctx"], dtype=np.float32)
    own = slice(hf * 2048, (hf + 1) * 2048)
    oth = slice((1 - hf) * 2048, (2 - hf) * 2048)
    xin = np.concatenate([x[bidx, own], x[bidx, oth], ctx[bidx]], axis=0)
    cvec = np.stack([np.asarray(inputs["c"], np.float32)[bidx], np.asarray(inputs["c_ctx"], np.float32)], 0)
    cvecT = np.ascontiguousarray(cvec.reshape(2, 8, 128).transpose(2, 0, 1))
    n = np.concatenate([np.arange(4096)[own], np.arange(4096)[oth]])
    rowpos = np.zeros((128, 34), np.float32)
    colpos = np.zeros((128, 34), np.float32)
    rowpos[:, :32] = (n // 64).reshape(32, 128).T
    colpos[:, :32] = (n % 64).reshape(32, 128).T
    return dict(xin=np.ascontiguousarray(xin), cvecT=cvecT, ident=np.eye(128, dtype=np.float32), rowpos=rowpos,
                colpos=colpos)


def kernel_unfused(**inputs):
    xins = [_common_inputs(inputs, c)["xin"] for c in range(NCORES)]
    for layer in range(2):
        resA = run_A(inputs, layer, xins)
        kv = assemble_kv(resA)
        resB = run_B(inputs, layer, xins, kv)
        xins = [np.ascontiguousarray(np.asarray(resB[c]["xout"], np.float32)) for c in range(NCORES)]
    out = np.empty((NB, SEQ, D), np.float32)
    for c in range(NCORES):
        out[c // 2, (c % 2) * 2048:(c % 2 + 1) * 2048] = xins[c][:2048]
    return out


def kernel(**inputs):
    nc = _prog("F", 0)
    names = ("w_mod", "b_mod", "g_attn", "w_in", "g_ckv", "w_ukv", "g_ka", "g_kb", "g_ffn", "g_cq", "w_uq", "g_qa", "g_qb",
             "w_oa", "w_ob", "w_out", "w_e_gate", "w_e_up", "w_e_down", "w_s_gate", "w_s_up", "w_s_down", "w_router")
    shared = {k: _f(inputs, k) for k in names}
    shared["b_router"] = _f(inputs, "b_router").reshape(1, 16)
    in_maps = []
    for c in range(NCORES):
        d = _fused_inputs(inputs, c)
        d.update(shared)
        in_maps.append(d)
    res = run_bass_kernel_spmd(nc, in_maps, core_ids=list(range(NCORES)))
    out = np.empty((NB, SEQ, D), np.float32)
    for c in range(NCORES):
        out[c // 2, (c % 2) * 2048:(c % 2 + 1) * 2048] = np.asarray(res.results[c]["xout"], np.float32)
    return out
```
